# Optimizing a Trainium2 kernel written in Bass

```python
import math
import jax
import jax.numpy as jnp
from jax import lax
import numpy as np

D_MODEL = 2048
BATCH = 8
SEQ = 2048
DEPTH = 2

N_MIXERS = 2
N_EVEN = (DEPTH + 1) // 2
N_ODD = DEPTH // 2
CONV_WIDTH = 3
N_HEADS = 16
HEAD_DIM = D_MODEL // N_HEADS
MOBA_BLOCK = 256
MOBA_TOPK = 3
Q_CHUNK = 16
ROPE_THETA = 10000.0
D_FF_DENSE = 5632
N_EXPERTS = 8
EXPERT_TOPK = 2
D_FF_EXPERT = 7168
EXPERT_ROW_BLOCK = 512
NORM_EPS = 1e-6
NEG_INF = -1e30
MOD_STD = 0.2

kernel_name = "hybrid_shortconv_moba_moe_adaln"


def rms_norm(x, gain):
    xf = x.astype(jnp.float32)
    y = xf * lax.rsqrt(jnp.mean(xf * xf, axis=-1, keepdims=True) + NORM_EPS)
    return (y * gain.astype(jnp.float32)).astype(x.dtype)


def modulate(h, shift, scale):
    return h * (1.0 + scale[:, None, :]) + shift[:, None, :]


def swiglu(h, w_gate, w_up, w_down):
    return (jax.nn.silu(h @ w_gate) * (h @ w_up)) @ w_down


def rope(x, positions):
    half = HEAD_DIM // 2
    inv_freq = jnp.exp(-math.log(ROPE_THETA) * jnp.arange(half, dtype=jnp.float32) / half)
    ang = positions.astype(jnp.float32)[:, None] * inv_freq[None, :]
    cos, sin = jnp.cos(ang), jnp.sin(ang)
    xf = x.astype(jnp.float32)
    x1, x2 = xf[..., :half], xf[..., half:]
    return jnp.concatenate([x1 * cos - x2 * sin, x2 * cos + x1 * sin], axis=-1).astype(x.dtype)


def short_conv_mixer(h, w_in, conv_w, w_out):
    b_gate, c_gate, v = jnp.split(h @ w_in, 3, axis=-1)
    u = c_gate * v
    u = lax.conv_general_dilated(
        u, conv_w[:, None, :].astype(u.dtype), window_strides=(1,),
        padding=[(CONV_WIDTH - 1, 0)], dimension_numbers=("NWC", "WIO", "NWC"),
        feature_group_count=u.shape[-1])
    return (b_gate * u) @ w_out


def moba_attention(h, w_qkv, q_gain, k_gain, w_out):
    B, S, _ = h.shape
    qkv = (h @ w_qkv).reshape(B, S, 3, N_HEADS, HEAD_DIM)
    q, k, v = (jnp.transpose(qkv[:, :, i], (0, 2, 1, 3)) for i in range(3))
    positions = jnp.arange(S)
    q = rope(rms_norm(q, q_gain), positions)
    k = rope(rms_norm(k, k_gain), positions)
    n_blk = -(-S // MOBA_BLOCK)
    pad = n_blk * MOBA_BLOCK - S
    kb = jnp.pad(k, ((0, 0), (0, 0), (0, pad), (0, 0))).reshape(B, N_HEADS, n_blk, MOBA_BLOCK, HEAD_DIM)
    vb = jnp.pad(v, ((0, 0), (0, 0), (0, pad), (0, 0))).reshape(B, N_HEADS, n_blk, MOBA_BLOCK, HEAD_DIM)
    k_mean = jnp.mean(kb.astype(jnp.float32), axis=3)
    n_sel = min(MOBA_TOPK, n_blk - 1)
    scale = HEAD_DIM ** -0.5
    n_chunks = S // Q_CHUNK
    q_chunks = q.reshape(B, N_HEADS, n_chunks, Q_CHUNK, HEAD_DIM).transpose(2, 0, 1, 3, 4)
    b_idx = jnp.arange(B)[:, None, None, None]
    h_idx = jnp.arange(N_HEADS)[None, :, None, None]
    blk_ids = jnp.arange(n_blk)
    key_off = jnp.arange(MOBA_BLOCK)

    def attend_chunk(args):
        q_c, c_idx = args
        q_pos = c_idx * Q_CHUNK + jnp.arange(Q_CHUNK)
        own = (c_idx * Q_CHUNK) // MOBA_BLOCK
        k_own = lax.dynamic_index_in_dim(kb, own, axis=2, keepdims=False)
        v_own = lax.dynamic_index_in_dim(vb, own, axis=2, keepdims=False)
        s_own = jnp.einsum("bhqd,bhkd->bhqk", q_c, k_own).astype(jnp.float32) * scale
        s_own = jnp.where(own * MOBA_BLOCK + key_off[None, :] <= q_pos[:, None], s_own, NEG_INF)
        if n_sel == 0:
            p_own = jax.nn.softmax(s_own, axis=-1).astype(v_own.dtype)
            return jnp.einsum("bhqk,bhkd->bhqd", p_own, v_own)
        gate = jnp.einsum("bhqd,bhnd->bhqn", q_c.astype(jnp.float32), k_mean)
        gate = jnp.where(blk_ids < own, gate, NEG_INF)
        _, sel = lax.top_k(gate, n_sel)
        sel_ok = sel < own
        k_sel = kb[b_idx, h_idx, sel]
        v_sel = vb[b_idx, h_idx, sel]
        s_sel = jnp.einsum("bhqd,bhqnkd->bhqnk", q_c, k_sel).astype(jnp.float32) * scale
        s_sel = jnp.where(sel_ok[..., None], s_sel, NEG_INF)
        s_all = jnp.concatenate([s_sel.reshape(B, N_HEADS, Q_CHUNK, n_sel * MOBA_BLOCK), s_own], axis=-1)
        p = jax.nn.softmax(s_all, axis=-1).astype(v_own.dtype)
        p_sel = p[..., :n_sel * MOBA_BLOCK].reshape(B, N_HEADS, Q_CHUNK, n_sel, MOBA_BLOCK)
        p_own = p[..., n_sel * MOBA_BLOCK:]
        return (jnp.einsum("bhqnk,bhqnkd->bhqd", p_sel, v_sel)
                + jnp.einsum("bhqk,bhkd->bhqd", p_own, v_own))

    o = lax.map(attend_chunk, (q_chunks, jnp.arange(n_chunks)))
    o = o.transpose(1, 0, 3, 2, 4).reshape(B, S, D_MODEL)
    return o @ w_out


def moe_swiglu(h, w_router, b_router, w_gate, w_up, w_down):
    B, S, D = h.shape
    tok = h.reshape(B * S, D)
    n_tok = B * S
    n_assign = n_tok * EXPERT_TOPK
    logits = (tok @ w_router).astype(jnp.float32) + b_router.astype(jnp.float32)
    top_logit, top_idx = lax.top_k(logits, EXPERT_TOPK)
    top_w = jax.nn.softmax(top_logit, axis=-1)
    flat_e = top_idx.reshape(-1)
    flat_tok = jnp.repeat(jnp.arange(n_tok, dtype=jnp.int32), EXPERT_TOPK)
    flat_w = top_w.reshape(-1)
    counts = jnp.bincount(flat_e, length=N_EXPERTS)
    padded = (counts + EXPERT_ROW_BLOCK - 1) // EXPERT_ROW_BLOCK * EXPERT_ROW_BLOCK
    pad_end = jnp.cumsum(padded)
    pad_start = pad_end - padded
    start = jnp.cumsum(counts) - counts
    order = jnp.argsort(flat_e)
    sorted_e = flat_e[order]
    dest = pad_start[sorted_e] + jnp.arange(n_assign) - start[sorted_e]
    n_blocks = -(-n_assign // EXPERT_ROW_BLOCK) + N_EXPERTS
    n_rows = n_blocks * EXPERT_ROW_BLOCK
    row_tok = jnp.zeros((n_rows,), jnp.int32).at[dest].set(flat_tok[order])
    row_w = jnp.zeros((n_rows,), jnp.float32).at[dest].set(flat_w[order])
    block_e = jnp.minimum(
        jnp.searchsorted(pad_end, jnp.arange(n_blocks) * EXPERT_ROW_BLOCK, side="right"),
        N_EXPERTS - 1)
    xs = tok[row_tok].reshape(n_blocks, EXPERT_ROW_BLOCK, D)

    def expert_block(args):
        x_b, e = args
        return swiglu(x_b, w_gate[e], w_up[e], w_down[e])

    ys = lax.map(expert_block, (xs, block_e)).reshape(n_rows, D)
    out = jnp.zeros_like(tok).at[row_tok].add(ys * row_w[:, None].astype(ys.dtype))
    return out.reshape(B, S, D)


def setup_inputs(seed: int = 0) -> dict:
    key = jax.random.key(seed)
    ks = jax.random.split(key, 24)
    D = D_MODEL

    def nrm(k, shape, std):
        return jax.random.normal(k, shape, jnp.float32) * std

    return {
        "x": nrm(ks[0], (BATCH, SEQ, D), 1.0),
        "c": nrm(ks[1], (BATCH, D), 1.0),
        "mod_w": nrm(ks[2], (DEPTH, D, 6 * D), MOD_STD * D ** -0.5),
        "mod_b": nrm(ks[3], (DEPTH, 6 * D), 0.02),
        "norm_mix": 1.0 + nrm(ks[4], (DEPTH, D), 0.05),
        "norm_ffn": 1.0 + nrm(ks[5], (DEPTH, D), 0.05),
        "conv_in": nrm(ks[6], (N_EVEN, D, 3 * D), D ** -0.5),
        "conv_w": nrm(ks[7], (N_EVEN, CONV_WIDTH, D), CONV_WIDTH ** -0.5),
        "conv_out": nrm(ks[8], (N_EVEN, D, D), D ** -0.5),
        "ffn_gate": nrm(ks[9], (N_EVEN, D, D_FF_DENSE), D ** -0.5),
        "ffn_up": nrm(ks[10], (N_EVEN, D, D_FF_DENSE), D ** -0.5),
        "ffn_down": nrm(ks[11], (N_EVEN, D_FF_DENSE, D), D_FF_DENSE ** -0.5),
        "qkv_w": nrm(ks[12], (N_ODD, D, 3 * D), D ** -0.5),
        "q_norm": 1.0 + nrm(ks[13], (N_ODD, HEAD_DIM), 0.05),
        "k_norm": 1.0 + nrm(ks[14], (N_ODD, HEAD_DIM), 0.05),
        "attn_out": nrm(ks[15], (N_ODD, D, D), D ** -0.5),
        "router_w": nrm(ks[16], (N_ODD, D, N_EXPERTS), D ** -0.5),
        "router_b": nrm(ks[17], (N_ODD, N_EXPERTS), 0.01),
        "exp_gate": nrm(ks[18], (N_ODD, N_EXPERTS, D, D_FF_EXPERT), D ** -0.5),
        "exp_up": nrm(ks[19], (N_ODD, N_EXPERTS, D, D_FF_EXPERT), D ** -0.5),
        "exp_down": nrm(ks[20], (N_ODD, N_EXPERTS, D_FF_EXPERT, D), D_FF_EXPERT ** -0.5),
    }


def reference(x, c, mod_w, mod_b, norm_mix, norm_ffn, conv_in, conv_w, conv_out,
              ffn_gate, ffn_up, ffn_down, qkv_w, q_norm, k_norm, attn_out,
              router_w, router_b, exp_gate, exp_up, exp_down):
    c_act = jax.nn.silu(c)
    for i in range(DEPTH):
        j = i // N_MIXERS
        mod = c_act @ mod_w[i] + mod_b[i]
        sh_m, sc_m, g_m, sh_f, sc_f, g_f = jnp.split(mod, 6, axis=-1)
        h = modulate(rms_norm(x, norm_mix[i]), sh_m, sc_m)
        if i % N_MIXERS == 0:
            y = short_conv_mixer(h, conv_in[j], conv_w[j], conv_out[j])
        else:
            y = moba_attention(h, qkv_w[j], q_norm[j], k_norm[j], attn_out[j])
        x = x + g_m[:, None, :] * y
        h = modulate(rms_norm(x, norm_ffn[i]), sh_f, sc_f)
        if i % 2 == 0:
            y = swiglu(h, ffn_gate[j], ffn_up[j], ffn_down[j])
        else:
            y = moe_swiglu(h, router_w[j], router_b[j], exp_gate[j], exp_up[j], exp_down[j])
        x = x + g_f[:, None, :] * y
    return x
```

```python
import contextlib
import math
import numpy as np
import concourse.bass as bass
import concourse.mybir as mybir
from concourse.bass_utils import run_bass_kernel_spmd

F32 = mybir.dt.float32
BF16 = mybir.dt.bfloat16
AF = mybir.ActivationFunctionType
ALU = mybir.AluOpType
AX = mybir.AxisListType

T = 2048
D = 2048
KC = 16
NH = 16
HD = 128
BLK = 256
NBLK = 8
F_DENSE = 5632
F_EXP = 7168
NE = 8
EPS = 1e-6
ATT_SCALE = HD ** -0.5
NEG = -30000.0

ENGINES = ("pe", "act", "dve", "pool", "sp")
SIG_LIMIT = 20000
N_DMA_SEMS = {"sp": 24, "act": 4, "pool": 12}


class Buf:
    __slots__ = ("last_w", "readers")

    def __init__(self):
        self.last_w = None
        self.readers = {}


class Op:
    __slots__ = ("idx", "eng", "fn", "is_dma", "deps", "needs_sig", "sig", "dma_sem", "dma_val", "dma_prev")

    def __init__(self, idx, eng, fn, is_dma):
        self.idx = idx
        self.eng = eng
        self.fn = fn
        self.is_dma = is_dma
        self.deps = {}
        self.needs_sig = False
        self.sig = None
        self.dma_sem = None
        self.dma_val = 0
        self.dma_prev = None


class Sched:
    def __init__(self, nc):
        self.nc = nc
        self.ops = []
        self.last_compute = {}
        self.out_dmas = []

    def add(self, eng, fn, reads=(), writes=(), dma=False):
        op = Op(len(self.ops), eng, fn, dma)
        deps = op.deps

        def dep(o, raw):
            if o is None:
                return
            key = (o.idx if o.is_dma else o.eng, raw)
            cur = deps.get(key)
            if cur is None or cur.idx < o.idx:
                deps[key] = o

        for b in reads:
            dep(b.last_w, True)
        for b in writes:
            dep(b.last_w, True)
            for r in b.readers.values():
                dep(r, False)
        for b in reads:
            b.readers[("d", op.idx) if dma else eng] = op
        for b in writes:
            b.last_w = op
            b.readers = {}
        self.ops.append(op)
        if dma:
            self.out_dmas.append(op)
        else:
            self.last_compute[eng] = op
        return op

    def dma(self, q, out, in_, reads=(), writes=()):
        return self.add(q, lambda e: e.dma_start(out=out, in_=in_), reads, writes, dma=True)

    def barrier(self):
        last = dict(self.last_compute)
        dmas = list(self.out_dmas)
        for e in ENGINES:
            op = Op(len(self.ops), e, None, False)
            for e2, o in last.items():
                if e2 != e:
                    op.deps[(e2, True)] = o
            for o in dmas:
                op.deps[(o.idx, True)] = o
            self.ops.append(op)
        self.out_dmas = []

    def emit(self):
        nc = self.nc
        self.barrier()
        ops = self.ops
        for op in ops:
            for ((_k, raw), o) in op.deps.items():
                if o.is_dma:
                    continue
                if o.eng == op.eng and not op.is_dma and (o.eng == "pe" or not raw):
                    continue
                o.needs_sig = True
        sig_cnt = {e: 0 for e in ENGINES}
        dma_cnt = {q: 0 for q in N_DMA_SEMS}
        dma_last = {}
        for op in ops:
            if op.is_dma:
                q = op.eng
                slot = dma_cnt[q] % N_DMA_SEMS[q]
                dma_cnt[q] += 1
                key = (q, slot)
                prev = dma_last.get(key)
                op.dma_prev = prev
                op.dma_sem = f"d_{q}_{slot}"
                op.dma_val = (prev.dma_val if prev else 0) + 16
                dma_last[key] = op
            elif op.needs_sig:
                sig_cnt[op.eng] += 1
                op.sig = sig_cnt[op.eng]
        sems = {}
        ctx = []

        def getsem(name):
            if name not in sems:
                g = nc.semaphore(name)
                sems[name] = g.__enter__()
                ctx.append(g)
            return sems[name]

        for e in ENGINES:
            for k in range((sig_cnt[e] + SIG_LIMIT - 1) // SIG_LIMIT):
                getsem(f"s_{e}_{k}")
        for q, n in N_DMA_SEMS.items():
            for s in range(min(n, dma_cnt[q])):
                getsem(f"d_{q}_{s}")
        per_eng = {e: [] for e in ENGINES}
        for op in ops:
            per_eng[op.eng].append(op)

        def run_engine(ename, eng):
            waited = {}

            def wait(nm, val):
                if waited.get(nm, 0) >= val:
                    return
                waited[nm] = val
                eng.wait_ge(sems[nm], val)

            for op in per_eng[ename]:
                for ((_k, raw), o) in op.deps.items():
                    if o.is_dma:
                        wait(o.dma_sem, o.dma_val)
                    else:
                        if o.eng == op.eng and not op.is_dma and (o.eng == "pe" or not raw):
                            continue
                        k = (o.sig - 1) // SIG_LIMIT
                        wait(f"s_{o.eng}_{k}", o.sig - k * SIG_LIMIT)
                if op.is_dma and op.dma_prev is not None:
                    wait(op.dma_sem, op.dma_prev.dma_val)
                if op.fn is None:
                    continue
                ins = op.fn(eng)
                if op.is_dma:
                    ins.then_inc(sems[op.dma_sem], 16)
                elif op.sig is not None:
                    k = (op.sig - 1) // SIG_LIMIT
                    ins.then_inc(sems[f"s_{op.eng}_{k}"], 1)

        with nc.Block() as block:
            @block.tensor
            def _(e):
                run_engine("pe", e)

            @block.scalar
            def _(e):
                run_engine("act", e)

            @block.vector
            def _(e):
                run_engine("dve", e)

            @block.gpsimd
            def _(e):
                run_engine("pool", e)

            @block.sync
            def _(e):
                run_engine("sp", e)
        for g in reversed(ctx):
            g.__exit__(None, None, None)
        return {e: len(per_eng[e]) for e in ENGINES}


class TB:
    def __init__(self, t, nb=1):
        self.t = t
        self.b = [Buf() for _ in range(nb)]

    def __getitem__(self, k):
        return self.t[k]


class _HTView:
    def __init__(self, tb):
        self.tb = tb
        self.b = _AnyIdx(tb.b[0])

    def __getitem__(self, k):
        p, kc, tsl = k
        return self.tb.t[p, kc, 0:512]


class _AnyIdx:
    def __init__(self, b):
        self._b = b

    def __getitem__(self, i):
        return self._b

class Prog:
    def __init__(self, stop_after=None):
        self.stop_after = stop_after
        self.nc = bass.Bass("TRN2", target_bir_lowering=False)
        self.S = Sched(self.nc)
        self.uid = 0

    def din(self, name, shape, dt=F32):
        return self.nc.dram_tensor(name, list(shape), dt, kind="ExternalInput").ap()

    def dscr(self, name, shape, dt=F32):
        return self.nc.dram_tensor(name, list(shape), dt, kind="Internal").ap()

    def sb(self, es, shape, dt, nb=1):
        self.uid += 1
        return TB(es.enter_context(self.nc.sbuf_tensor(f"sb{self.uid}", list(shape), dt)), nb)

    def ps(self, es, shape, dt=F32, nb=1):
        self.uid += 1
        ncol = 512 if dt == F32 else 1024
        full = es.enter_context(self.nc.psum_tensor(f"ps{self.uid}", [128, ncol], dt))
        n = 1
        for d in shape[1:]:
            n *= d
        v = full[:, 0:n]
        if len(shape) == 3:
            v = v.rearrange("p (a b) -> p a b", a=shape[1])
        return TB(v, nb)

    def _bc_reg(self, eng, val):
        if getattr(self, "_bcr", None) is None:
            self._bcr = eng.to_reg(val)
        return self._bcr

    def mm(self, out, lhsT, rhs, start, stop, reads, writes):
        self.S.add("pe", lambda e: e.matmul(out, lhsT, rhs, start=start, stop=stop), reads, writes)

    def tr(self, out, in_, ident, reads, writes):
        self.S.add("pe", lambda e: e.transpose(out, in_, ident), reads, writes)

    def act(self, out, in_, func, reads, writes, bias=None, scale=None, accum_out=None):
        kw = {}
        if bias is not None:
            kw["bias"] = bias
        if scale is not None:
            kw["scale"] = scale
        if accum_out is not None:
            kw["accum_out"] = accum_out
        self.S.add("act", lambda e: e.activation(out, in_, func, **kw), reads, writes)

    def copy(self, eng, out, in_, reads, writes):
        if eng == "act":
            self.S.add("act", lambda e: e.copy(out, in_), reads, writes)
        else:
            self.S.add(eng, lambda e: e.tensor_copy(out, in_), reads, writes)

    def tt(self, out, in0, in1, op, reads, writes, eng="dve"):
        self.S.add(eng, lambda e: e.tensor_tensor(out, in0, in1, op), reads, writes)

    def ts(self, out, in0, s1, s2, op0, op1, reads, writes, eng="dve"):
        if op1 is None:
            self.S.add(eng, lambda e: e.tensor_scalar(out, in0, s1, None, op0), reads, writes)
        else:
            self.S.add(eng, lambda e: e.tensor_scalar(out, in0, s1, s2, op0, op1), reads, writes)

    def stt(self, out, in0, scalar, in1, op0, op1, reads, writes, eng="dve"):
        self.S.add(eng, lambda e: e.scalar_tensor_tensor(out, in0, scalar, in1, op0, op1), reads, writes)

    def build(self):
        nc, S = self.nc, self.S
        stop = self.stop_after
        I = {}
        I["x"] = self.din("x", [T, D])
        I["cT"] = self.din("cT", [128, KC])
        I["mod_w"] = self.din("mod_w", [2, D, 6 * D])
        I["mod_bT"] = self.din("mod_bT", [128, 2, 96])
        I["gains"] = self.din("gains", [128, 4, KC])
        I["ident"] = self.din("ident", [128, 128])
        I["conv_in"] = self.din("conv_in", [D, 3 * D])
        I["conv_wT"] = self.din("conv_wT", [128, 3, KC])
        I["conv_out"] = self.din("conv_out", [D, D])
        I["ffn_gate"] = self.din("ffn_gate", [D, F_DENSE])
        I["ffn_up"] = self.din("ffn_up", [D, F_DENSE])
        I["ffn_down"] = self.din("ffn_down", [F_DENSE, D])
        if stop is None or stop >= 3:
            I["qkv_w"] = self.din("qkv_w", [D, 3 * D])
            I["qk_gain"] = self.din("qk_gain", [128, 4])
            I["ropeT"] = self.din("ropeT", [128, 2, T])
            I["tri"] = self.din("tri", [128, 256])
            I["pastmask"] = self.din("pastmask", [128, 8, 8])
            I["attn_out"] = self.din("attn_out", [D, D])
        if stop is None or stop >= 4:
            I["router_w"] = self.din("router_w", [128, KC, NE])
            I["router_b"] = self.din("router_b", [128, NE])
            I["ustrict"] = self.din("ustrict", [128, 128])
            I["exp_gate"] = self.din("exp_gate", [NE, D, F_EXP])
            I["exp_up"] = self.din("exp_up", [NE, D, F_EXP])
            I["exp_down"] = self.din("exp_down", [NE, F_EXP, D])
        self.I = I
        self.out = nc.dram_tensor("out", [T, D], F32, kind="ExternalOutput").ap()
        self.R = [self.dscr("R0", [128, KC, T]), self.dscr("R1", [128, KC, T])]
        self.Rb = [[Buf() for _ in range(KC)], [Buf() for _ in range(KC)]]

        with contextlib.ExitStack() as top:
            self.top = top
            self.ident = self.sb(top, [128, 128], F32)
            self.identb = self.sb(top, [128, 128], BF16)
            self.onesD = self.sb(top, [128, 128], BF16)
            self.onesH = self.sb(top, [128, 128], BF16)
            self.ones32 = self.sb(top, [128, 128], F32)
            self.epsT = self.sb(top, [128, 1], F32)
            self.modT = self.sb(top, [128, 2, 96], F32)
            self.Aprm = self.sb(top, [128, 2, 2, KC], F32)
            self.gains = self.sb(top, [128, 4, KC], F32)
            self.convw = self.sb(top, [128, 3, KC], F32)
            self.phase_setup()
            S.barrier()
            self.phase_mod()
            S.barrier()
            with contextlib.ExitStack() as es:
                hT = self.sb(es, [128, KC, T], BF16, nb=4)
                self.phase_prenorm(hT, True, None, 0, self.Aprm[:, 0, 0, :], self.modT[:, 0, 0:16])
                S.barrier()
                cur = 0
                if stop is None or stop >= 1:
                    zT = self.sb(es, [128, KC, T], BF16, nb=1)
                    self.phase_conv(hT, zT)
                    S.barrier()
                    cur = 1
            if stop is None or stop >= 2:
                self.phase_ffn(R_in=1, R_out=0, A=self.Aprm[:, 0, 1, :], B=self.modT[:, 0, 48:64], G=self.modT[:, 0, 80:96],
                               experts=[(I["ffn_gate"], I["ffn_up"], I["ffn_down"], F_DENSE // 128)], FG=4, moe=False)
                S.barrier()
                cur = 0
            if stop is None or stop >= 3:
                self.phase_attn(R_in=0, R_out=1)
                S.barrier()
                cur = 1
            if stop is None or stop >= 4:
                ex = [(I["exp_gate"][e], I["exp_up"][e], I["exp_down"][e], F_EXP // 128) for e in range(NE)]
                self.phase_moe_sparse(R_in=1, A=self.Aprm[:, 1, 1, :], B=self.modT[:, 1, 48:64], G=self.modT[:, 1, 80:96], experts=ex)
            else:
                self.phase_final(cur)
            counts = S.emit()
        self.counts = counts
        return nc

    def phase_setup(self):
        S, I = self.S, self.I
        b = Buf()
        S.dma("sp", self.ident[:], I["ident"], writes=[b])
        S.dma("sp", self.gains[:], I["gains"], writes=[Buf()])
        S.dma("sp", self.convw[:], I["conv_wT"], writes=[Buf()])
        S.add("dve", lambda e: e.tensor_copy(self.identb[:], self.ident[:]), reads=[b], writes=[Buf()])
        S.add("dve", lambda e: e.memset(self.onesD[:], 1.0 / D), writes=[Buf()])
        S.add("dve", lambda e: e.memset(self.onesH[:], 1.0 / HD), writes=[Buf()])
        S.add("dve", lambda e: e.memset(self.ones32[:], 1.0), writes=[Buf()])
        S.add("dve", lambda e: e.memset(self.epsT[:], EPS), writes=[Buf()])

    def phase_mod(self):
        S, I = self.S, self.I
        with contextlib.ExitStack() as es:
            c_sb = self.sb(es, [128, KC], F32)
            cact = self.sb(es, [128, KC], BF16)
            mb = self.sb(es, [128, 2, 96], F32)
            wts = [self.sb(es, [128, KC, 512], BF16) for _ in range(4)]
            pm = [self.ps(es, [128, 96], F32) for _ in range(2)]
            S.dma("sp", c_sb[:], I["cT"], writes=c_sb.b)
            S.dma("sp", mb[:], I["mod_bT"], writes=mb.b)
            self.act(cact[:], c_sb[:], AF.Silu, reads=c_sb.b, writes=cact.b)
            n = 0
            for i in range(2):
                for cb in range(24):
                    w = wts[n % 4]
                    n += 1
                    src = I["mod_w"][i][:, cb * 512:(cb + 1) * 512].rearrange("(kc p) n -> p kc n", p=128)
                    S.dma("pool", w[:], src, writes=w.b)
                    for jj in range(4):
                        j = cb * 4 + jj
                        for kc in range(KC):
                            self.mm(pm[i][:, j:j + 1], w[:, kc, jj * 128:(jj + 1) * 128], cact[:, kc:kc + 1],
                                    kc == 0, kc == KC - 1, reads=w.b + cact.b, writes=pm[i].b)
                self.tt(self.modT[:, i, :], pm[i][:], mb[:, i, :], ALU.add, reads=pm[i].b + mb.b, writes=self.modT.b)
                for k, (sc_lo, gi) in enumerate(((16, 2 * i), (64, 2 * i + 1))):
                    self.stt(self.Aprm[:, i, k, :], self.modT[:, i, sc_lo:sc_lo + 16], 1.0, self.gains[:, gi, :],
                             ALU.add, ALU.mult, reads=self.modT.b, writes=self.Aprm.b)

    def norm_tile(self, xT, A, B, out_ap_fn, out_bufs, sq, rstd, tmps, ps_ss, h32_cb=None):
        self.act(sq[:], xT[:], AF.Square, reads=xT.b, writes=sq.b)
        for kc in range(KC):
            self.mm(ps_ss[:], self.onesD[:], sq[:, kc, :], kc == 0, kc == KC - 1, reads=sq.b, writes=ps_ss.b)
        self.act(rstd[:], ps_ss[:], AF.Ln, reads=ps_ss.b, writes=rstd.b, bias=self.epsT[:], scale=1.0)
        self.act(rstd[:], rstd[:], AF.Exp, reads=rstd.b, writes=rstd.b, scale=-0.5)
        for kc in range(KC):
            tmp = tmps[kc % len(tmps)]
            self.stt(tmp[:], xT[:, kc, :], A[:, kc:kc + 1], rstd[:], ALU.mult, ALU.mult, reads=xT.b + rstd.b, writes=tmp.b)
            if h32_cb is None:
                self.act(out_ap_fn(kc), tmp[:], AF.Identity, reads=tmp.b, writes=out_bufs, bias=B[:, kc:kc + 1], scale=1.0)
            else:
                h32_cb(kc, tmp, B[:, kc:kc + 1], out_ap_fn(kc), out_bufs)

    def phase_prenorm(self, hT, src_tokmajor, R_src, R_dst, A, B, t_lo=0, n_tt=4, h32_cb=None, tt_done_cb=None):
        S, I = self.S, self.I
        with contextlib.ExitStack() as es:
            xT = self.sb(es, [128, KC, 512], F32)
            sq = self.sb(es, [128, KC, 512], BF16)
            rstd = self.sb(es, [128, 512], F32)
            tmps = [self.sb(es, [128, 512], F32) for _ in range(2)]
            ps_ss = self.ps(es, [128, 512], F32)
            if src_tokmajor:
                xin = self.sb(es, [128, 4, D], F32)
                ps_tr = [self.ps(es, [128, 512], F32) for _ in range(2)]
            for ti in range(n_tt):
                tt = t_lo + ti
                tsl = slice(tt * 512, (tt + 1) * 512)
                if src_tokmajor:
                    S.dma("sp", xin[:], I["x"][tsl, :].rearrange("(s p) d -> p s d", p=128), writes=xin.b)
                    for kc in range(KC):
                        p = ps_tr[kc % 2]
                        for s in range(4):
                            self.tr(p[:, s * 128:(s + 1) * 128], xin[:, s, kc * 128:(kc + 1) * 128], self.ident[:], reads=xin.b, writes=p.b)
                        self.copy("act" if kc % 2 else "dve", xT[:, kc, :], p[:], reads=p.b, writes=xT.b)
                    S.dma("sp", self.R[R_dst][:, :, tsl], xT[:], reads=xT.b, writes=self.Rb[R_dst])
                else:
                    S.dma("sp", xT[:], self.R[R_src][:, :, tsl], reads=self.Rb[R_src], writes=xT.b)
                lt = ti
                self.norm_tile(xT, A, B, lambda kc: hT[:, kc, lt * 512:(lt + 1) * 512], [hT.b[lt]], sq, rstd, tmps, ps_ss, h32_cb=h32_cb)
                if tt_done_cb is not None:
                    tt_done_cb(ti)

    def wload(self, wt, src2d, c0, ncols):
        src = src2d[:, c0:c0 + ncols].rearrange("(kc p) n -> p kc n", p=128)
        self.S.dma("pool", wt[:, :, 0:ncols], src, writes=wt.b)

    def phase_conv(self, hT, zT):
        S, I = self.S, self.I
        Gm = self.modT[:, 0, 32:48]
        with contextlib.ExitStack() as es:
            wp = [self.sb(es, [128, KC, 128], BF16) for _ in range(6)]
            u = self.sb(es, [128, T + 2], F32)
            bsb = self.sb(es, [128, T], F32)
            t0 = self.sb(es, [128, T], F32)
            csb = [self.sb(es, [128, 512], F32) for _ in range(2)]
            pss = [self.ps(es, [128, 512], F32) for _ in range(6)]
            S.add("dve", lambda e: e.memset(u[:, 0:2], 0.0), writes=u.b)
            n = 0
            for g in range(8):
                for jj in range(2):
                    j = 2 * g + jj
                    wb_, wc_, wv_ = wp[(j % 2) * 3:(j % 2) * 3 + 3]
                    for k, w_ in enumerate((wb_, wc_, wv_)):
                        self.wload(w_, I["conv_in"], k * D + j * 128, 128)
                    cs = slice(0, 128)
                    for tt in range(4):
                        tsl = slice(tt * 512, (tt + 1) * 512)
                        pc, pv, pb = pss[(n % 2) * 3:(n % 2) * 3 + 3]
                        n += 1
                        for (pt, w) in ((pc, wc_), (pv, wv_), (pb, wb_)):
                            for kc in range(KC):
                                self.mm(pt[:], w[:, kc, cs], hT[:, kc, tsl], kc == 0, kc == KC - 1, reads=w.b + [hT.b[tt]], writes=pt.b)
                        c_ = csb[tt % 2]
                        self.copy("act", c_[:], pc[:], reads=pc.b, writes=c_.b)
                        self.tt(u[:, 2 + tt * 512:2 + (tt + 1) * 512], pv[:], c_[:], ALU.mult, reads=pv.b + c_.b, writes=u.b)
                        self.copy("act", bsb[:, tsl], pb[:], reads=pb.b, writes=bsb.b)
                    cw = self.convw
                    self.ts(t0[:], u[:, 2:T + 2], cw[:, 2, j:j + 1], None, ALU.mult, None, reads=u.b, writes=t0.b)
                    self.stt(t0[:], u[:, 1:T + 1], cw[:, 1, j:j + 1], t0[:], ALU.mult, ALU.add, reads=u.b + t0.b, writes=t0.b)
                    self.stt(t0[:], u[:, 0:T], cw[:, 0, j:j + 1], t0[:], ALU.mult, ALU.add, reads=u.b + t0.b, writes=t0.b)
                    self.tt(zT[:, j, :], bsb[:], t0[:], ALU.mult, reads=bsb.b + t0.b, writes=zT.b)
        S.barrier()
        self.phase_outproj(zT, I["conv_out"], Gm, R_in=0, R_out=1)

    def phase_outproj(self, zT, W, G, R_in, R_out):
        S = self.S
        with contextlib.ExitStack() as es:
            wp = [self.sb(es, [128, KC, 256], BF16) for _ in range(3)]
            xres = [self.sb(es, [128, T], F32) for _ in range(2)]
            xnew = [self.sb(es, [128, T], F32) for _ in range(2)]
            pss = [self.ps(es, [128, 512], F32) for _ in range(4)]
            n = 0
            for g in range(8):
                w = wp[g % 3]
                self.wload(w, W, g * 256, 256)
                for jj in range(2):
                    m = 2 * g + jj
                    xr, xn = xres[m % 2], xnew[m % 2]
                    S.dma("sp", xr[:], self.R[R_in][:, m, :], reads=[self.Rb[R_in][m]], writes=xr.b)
                    for tt in range(4):
                        tsl = slice(tt * 512, (tt + 1) * 512)
                        p = pss[n % 4]
                        n += 1
                        for kc in range(KC):
                            self.mm(p[:], w[:, kc, jj * 128:(jj + 1) * 128], zT[:, kc, tsl], kc == 0, kc == KC - 1, reads=w.b + zT.b, writes=p.b)
                        self.stt(xn[:, tsl], p[:], G[:, m:m + 1], xr[:, tsl], ALU.mult, ALU.add, reads=p.b + xr.b, writes=xn.b)
                    S.dma("sp", self.R[R_out][:, m, :], xn[:], reads=xn.b, writes=[self.Rb[R_out][m]])

    def phase_ffn(self, R_in, R_out, A, B, G, experts, FG, moe):
        S, I = self.S, self.I
        TG = 1024
        for half in range(2):
            with contextlib.ExitStack() as es:
                hTh = self.sb(es, [128, KC, TG], BF16, nb=2)
                wB = self.sb(es, [128, NE, TG], BF16, nb=2) if moe else None
                if moe:
                    self.moe_norm_route(hTh, wB, R_in, A, B, half)
                else:
                    self.phase_prenorm(hTh, False, R_in, None, A, B, t_lo=half * 2, n_tt=2)
                S.barrier()
                with contextlib.ExitStack() as es2:
                    acc = self.sb(es2, [128, KC, TG], F32, nb=KC * 2)
                    hid = [self.sb(es2, [128, FG, TG], BF16, nb=2) for _ in range(2)]
                    wp = [self.sb(es2, [128, KC * 256], BF16) for _ in range(4)]
                    sg = [self.sb(es2, [128, 512], F32) for _ in range(2)]
                    xres = [self.sb(es2, [128, TG], F32) for _ in range(2)]
                    xnew = [self.sb(es2, [128, TG], F32) for _ in range(2)]
                    pg = [self.ps(es2, [128, 512], F32) for _ in range(2)]
                    pu = [self.ps(es2, [128, 512], F32) for _ in range(2)]
                    pd = [self.ps(es2, [128, 512], F32) for _ in range(4)]
                    wi = 0
                    n = 0
                    nd = 0
                    first = True
                    for e, (Wg, Wu, Wd, F) in enumerate(experts):
                        for fg in range(F // FG):
                            hb = hid[fg % 2]
                            dts = []
                            for pr in range(FG // 2):
                                f0 = fg * FG + pr * 2
                                gt = wp[wi % 4]; wi += 1
                                ut = wp[wi % 4]; wi += 1
                                for (wt, W) in ((gt, Wg), (ut, Wu)):
                                    src = W[:, f0 * 128:(f0 + 2) * 128].rearrange("(kc p) n -> p kc n", p=128)
                                    S.dma("pool", wt[:].rearrange("p (kc n) -> p kc n", kc=KC), src, writes=wt.b)
                                for jj in range(2):
                                    fl = pr * 2 + jj
                                    for tt in range(2):
                                        tsl = slice(tt * 512, (tt + 1) * 512)
                                        p_g, p_u = pg[n % 2], pu[n % 2]
                                        s_ = sg[n % 2]
                                        n += 1
                                        for (pt, wt) in ((p_g, gt), (p_u, ut)):
                                            for kc in range(KC):
                                                c0 = kc * 256 + jj * 128
                                                self.mm(pt[:], wt[:, c0:c0 + 128], hTh[:, kc, tsl], kc == 0, kc == KC - 1,
                                                        reads=wt.b + [hTh.b[tt]], writes=pt.b)
                                        self.act(s_[:], p_g[:], AF.Silu, reads=p_g.b, writes=s_.b)
                                        if moe:
                                            self.tt(s_[:], s_[:], wB[:, e, tsl], ALU.mult, reads=s_.b + [wB.b[tt]], writes=s_.b)
                                        self.tt(hb[:, fl, tsl], p_u[:], s_[:], ALU.mult, reads=p_u.b + s_.b, writes=[hb.b[tt]])
                            for pr in range(FG // 2):
                                f0 = fg * FG + pr * 2
                                dt_ = wp[wi % 4]; wi += 1
                                src = Wd[f0 * 128:(f0 + 2) * 128, :].rearrange("(c p) n -> p c n", p=128)
                                S.dma("pool", dt_[:].rearrange("p (c n) -> p c n", c=2), src, writes=dt_.b)
                                dts.append(dt_)
                                if len(dts) == 2 or pr == FG // 2 - 1:
                                    pass
                            self._down_group(dts, hb, acc, pd, first, FG)
                            first = False
                    hsl = slice(half * TG, (half + 1) * TG)
                    for m in range(KC):
                        xr, xn = xres[m % 2], xnew[m % 2]
                        S.dma("sp", xr[:], self.R[R_in][:, m, hsl], reads=[self.Rb[R_in][m]], writes=xr.b)
                        self.stt(xn[:], acc[:, m, :], G[:, m:m + 1], xr[:], ALU.mult, ALU.add,
                                 reads=[acc.b[2 * m], acc.b[2 * m + 1]] + xr.b, writes=xn.b)
                        S.dma("sp", self.R[R_out][:, m, hsl], xn[:], reads=xn.b, writes=[self.Rb[R_out][m]])
            S.barrier()

    def _down_group(self, dts, hb, acc, pd, first, FG):
        if not hasattr(self, "_nd"):
            self._nd = 0
        for m in range(KC):
            for tt in range(2):
                tsl = slice(tt * 512, (tt + 1) * 512)
                p = pd[self._nd % 4]
                self._nd += 1
                for fl in range(FG):
                    d = dts[fl // 2]
                    c0 = (fl % 2) * D + m * 128
                    self.mm(p[:], d[:, c0:c0 + 128], hb[:, fl, tsl], fl == 0, fl == FG - 1, reads=d.b + [hb.b[tt]], writes=p.b)
                ab = [acc.b[2 * m + tt]]
                if first:
                    self.copy("dve", acc[:, m, tsl], p[:], reads=p.b, writes=ab)
                else:
                    self.tt(acc[:, m, tsl], p[:], acc[:, m, tsl], ALU.add, reads=p.b + ab, writes=ab)

    def moe_norm_route(self, hTh, wB, R_in, A, B, half):
        S, I = self.S, self.I
        with contextlib.ExitStack() as es:
            rw = self.sb(es, [128, KC, NE], F32)
            rb = self.sb(es, [128, NE], F32)
            h32 = [self.sb(es, [128, 512], F32) for _ in range(2)]
            lg = self.sb(es, [128, 4, NE], F32)
            mx8 = self.sb(es, [128, 8], F32)
            ntop = self.sb(es, [128, 1], F32)
            ex = self.sb(es, [128, NE], F32)
            msk = self.sb(es, [128, NE], F32)
            den = self.sb(es, [128, 1], F32)
            wts = self.sb(es, [128, 4, NE], F32)
            diag = [self.sb(es, [128, 128], F32) for _ in range(2)]
            ps_lg = [self.ps(es, [128, NE], F32) for _ in range(4)]
            ps_wb = [self.ps(es, [128, 512], F32) for _ in range(2)]
            S.dma("sp", rw[:], I["router_w"], writes=rw.b)
            S.dma("sp", rb[:], I["router_b"], writes=rb.b)
            state = {"n": 0}

            def h32_cb(kc, tmp, bias, out_ap, out_bufs):
                h = h32[state["n"] % 2]
                state["n"] += 1
                self.act(h[:], tmp[:], AF.Identity, reads=tmp.b, writes=h.b, bias=bias, scale=1.0)
                for s in range(4):
                    self.mm(ps_lg[s][:], h[:, s * 128:(s + 1) * 128], rw[:, kc, :], kc == 0, kc == KC - 1,
                            reads=h.b + rw.b, writes=ps_lg[s].b)
                self.copy("dve", out_ap, h[:], reads=h.b, writes=out_bufs)

            def tt_done(ti):
                for s in range(4):
                    self.tt(lg[:, s, :], ps_lg[s][:], rb[:], ALU.add, reads=ps_lg[s].b + rb.b, writes=lg.b)
                    S.add("dve", lambda e, s=s: e.max(mx8[:], lg[:, s, :]), reads=lg.b, writes=mx8.b)
                    self.ts(msk[:], lg[:, s, :], mx8[:, 1:2], None, ALU.is_ge, None, reads=lg.b + mx8.b, writes=msk.b)
                    self.ts(ntop[:], mx8[:, 0:1], -1.0, None, ALU.mult, None, reads=mx8.b, writes=ntop.b)
                    self.act(ex[:], lg[:, s, :], AF.Exp, reads=lg.b + ntop.b, writes=ex.b, bias=ntop[:], scale=1.0)
                    self.tt(ex[:], ex[:], msk[:], ALU.mult, reads=ex.b + msk.b, writes=ex.b)
                    S.add("dve", lambda e: e.reduce_sum(den[:], ex[:], AX.X), reads=ex.b, writes=den.b)
                    S.add("dve", lambda e: e.reciprocal(den[:], den[:]), reads=den.b, writes=den.b)
                    self.ts(wts[:, s, :], ex[:], den[:, 0:1], None, ALU.mult, None, reads=ex.b + den.b, writes=wts.b)
                k = 0
                for e_ in range(NE):
                    p = ps_wb[e_ % 2]
                    for s in range(4):
                        dg = diag[k % 2]
                        k += 1
                        self.ts(dg[:], self.ident[:], wts[:, s, e_:e_ + 1], None, ALU.mult, None, reads=wts.b, writes=dg.b)
                        self.mm(p[:, s * 128:(s + 1) * 128], self.ones32[:], dg[:], True, True, reads=dg.b, writes=p.b)
                    self.copy("act", wB[:, e_, ti * 512:(ti + 1) * 512], p[:], reads=p.b, writes=[wB.b[ti]])

            self.phase_prenorm(hTh, False, R_in, None, A, B, t_lo=half * 2, n_tt=2, h32_cb=h32_cb, tt_done_cb=tt_done)


    def phase_moe_sparse(self, R_in, A, B, G, experts):
        S, I = self.S, self.I
        CAP = 896
        BIG = 4096.0
        XG = [self.dscr(f"XG{e}", [CAP, D], BF16) for e in range(NE)]
        YG = [self.dscr(f"YG{e}", [CAP, D], F32) for e in range(NE)]
        XGz = [Buf() for _ in range(NE)]
        YGb = [Buf() for _ in range(NE)]
        I32 = mybir.dt.int32
        with contextlib.ExitStack() as es0:
            wts_all = self.sb(es0, [128, 16, NE], F32)
            idx_all = self.sb(es0, [128, 16, NE], I32)
            with contextlib.ExitStack() as es:
                rw = self.sb(es, [128, KC, NE], F32)
                rb = self.sb(es, [128, NE], F32)
                us = self.sb(es, [128, 128], F32)
                zt = self.sb(es, [128, D], BF16)
                run = self.sb(es, [128, NE], F32)
                h32 = [self.sb(es, [128, 512], F32) for _ in range(2)]
                hTt = self.sb(es, [128, KC, 512], BF16, nb=1)
                htok = [self.sb(es, [128, D], BF16) for _ in range(4)]
                lg = self.sb(es, [128, NE], F32)
                mx8 = self.sb(es, [128, 8], F32)
                ntop = self.sb(es, [128, 1], F32)
                ex = self.sb(es, [128, NE], F32)
                msk = self.sb(es, [128, NE], F32)
                den = self.sb(es, [128, 1], F32)
                posf = self.sb(es, [128, NE], F32)
                ps_lg = [self.ps(es, [128, NE], F32) for _ in range(4)]
                ps_pos = self.ps(es, [128, 2 * NE], F32)
                ps_th = [self.ps(es, [128, 1024], BF16) for _ in range(2)]
                S.dma("sp", rw[:], I["router_w"], writes=rw.b)
                S.dma("sp", rb[:], I["router_b"], writes=rb.b)
                S.dma("sp", us[:], I["ustrict"], writes=us.b)
                S.add("dve", lambda e: e.memset(zt[:], 0.0), writes=zt.b)
                S.add("dve", lambda e: e.memset(run[:], 0.0), writes=run.b)
                for e_ in range(NE):
                    for st in range(CAP // 128):
                        S.dma("sp", XG[e_][st * 128:(st + 1) * 128, :], zt[:], reads=zt.b, writes=[XGz[e_]])
                state = {"n": 0, "nth": 0}

                def h32_cb(kc, tmp, bias, out_ap, out_bufs):
                    h = h32[state["n"] % 2]
                    state["n"] += 1
                    self.act(h[:], tmp[:], AF.Identity, reads=tmp.b, writes=h.b, bias=bias, scale=1.0)
                    for s_ in range(4):
                        self.mm(ps_lg[s_][:], h[:, s_ * 128:(s_ + 1) * 128], rw[:, kc, :], kc == 0, kc == KC - 1,
                                reads=h.b + rw.b, writes=ps_lg[s_].b)
                    self.copy("dve", out_ap, h[:], reads=h.b, writes=out_bufs)

                def tt_done(ti):
                    for s_ in range(4):
                        g = ti * 4 + s_
                        self.tt(lg[:], ps_lg[s_][:], rb[:], ALU.add, reads=ps_lg[s_].b + rb.b, writes=lg.b)
                        S.add("dve", lambda e: e.max(mx8[:], lg[:]), reads=lg.b, writes=mx8.b)
                        self.ts(msk[:], lg[:], mx8[:, 1:2], None, ALU.is_ge, None, reads=lg.b + mx8.b, writes=msk.b)
                        self.ts(ntop[:], mx8[:, 0:1], -1.0, None, ALU.mult, None, reads=mx8.b, writes=ntop.b)
                        self.act(ex[:], lg[:], AF.Exp, reads=lg.b + ntop.b, writes=ex.b, bias=ntop[:], scale=1.0)
                        self.tt(ex[:], ex[:], msk[:], ALU.mult, reads=ex.b + msk.b, writes=ex.b)
                        S.add("dve", lambda e: e.reduce_sum(den[:], ex[:], AX.X), reads=ex.b, writes=den.b)
                        S.add("dve", lambda e: e.reciprocal(den[:], den[:]), reads=den.b, writes=den.b)
                        self.ts(wts_all[:, g, :], ex[:], den[:, 0:1], None, ALU.mult, None, reads=ex.b + den.b, writes=wts_all.b)
                        self.mm(ps_pos[:, 0:NE], us[:], msk[:], True, True, reads=us.b + msk.b, writes=ps_pos.b)
                        self.mm(ps_pos[:, NE:2 * NE], self.ones32[:], msk[:], True, True, reads=msk.b, writes=ps_pos.b)
                        self.tt(posf[:], ps_pos[:, 0:NE], run[:], ALU.add, reads=ps_pos.b + run.b, writes=posf.b)
                        self.stt(posf[:], posf[:], -BIG, msk[:], ALU.add, ALU.mult, reads=posf.b + msk.b, writes=posf.b)
                        self.ts(posf[:], posf[:], BIG, None, ALU.add, None, reads=posf.b, writes=posf.b)
                        self.copy("dve", idx_all[:, g, :], posf[:], reads=posf.b, writes=idx_all.b)
                        self.tt(run[:], run[:], ps_pos[:, NE:2 * NE], ALU.add, reads=run.b + ps_pos.b, writes=run.b)
                        ht = htok[s_]
                        for hf in range(2):
                            p = ps_th[state["nth"] % 2]
                            state["nth"] += 1
                            for i in range(8):
                                kc = hf * 8 + i
                                self.tr(p[:, i * 128:(i + 1) * 128], hTt[:, kc, s_ * 128:(s_ + 1) * 128], self.identb[:],
                                        reads=hTt.b, writes=p.b)
                            self.copy("act", ht[:, hf * 1024:(hf + 1) * 1024], p[:], reads=p.b, writes=ht.b)
                        for e_ in range(NE):
                            off = bass.IndirectOffsetOnAxis(ap=idx_all[:, g, e_:e_ + 1], axis=0)
                            S.add("pool", lambda eng, e_=e_, off=off, ht=ht: eng.indirect_dma_start(
                                out=XG[e_][:, :], out_offset=off, in_=ht[:, :], in_offset=None,
                                bounds_check=self._bc_reg(eng, CAP - 1), oob_is_err=False),
                                reads=ht.b + idx_all.b + [XGz[e_]], writes=(), dma=True)

                self.phase_prenorm(_HTView(hTt), False, R_in, None, A, B, t_lo=0, n_tt=4, h32_cb=h32_cb, tt_done_cb=tt_done)
            S.barrier()
            FG = 8
            with contextlib.ExitStack() as es2:
                xgT = self.sb(es2, [128, KC, CAP], BF16, nb=2)
                acc = self.sb(es2, [128, CAP // 128, D], F32, nb=32)
                hid = [self.sb(es2, [128, FG, CAP], BF16, nb=2) for _ in range(2)]
                NW = 4
                wp = [self.sb(es2, [128, KC * 512], BF16) for _ in range(NW)]
                sg = [self.sb(es2, [128, 512], F32) for _ in range(2)]
                xs = [self.sb(es2, [128, D], BF16) for _ in range(2)]
                pg = [self.ps(es2, [128, 512], F32) for _ in range(2)]
                pu = [self.ps(es2, [128, 512], F32) for _ in range(2)]
                pd = [self.ps(es2, [128, 512], F32) for _ in range(3)]
                pt = self.ps(es2, [128, 1024], BF16)
                wi = 0
                n = 0
                nd = 0
                for e_, (Wg, Wu, Wd, F) in enumerate(experts):
                    for st in range(CAP // 128):
                        x_ = xs[st % 2]
                        S.dma("sp", x_[:], XG[e_][st * 128:(st + 1) * 128, :], writes=x_.b)
                        for hf in range(2):
                            for i in range(8):
                                kc = hf * 8 + i
                                self.tr(pt[:, i * 128:(i + 1) * 128], x_[:, kc * 128:(kc + 1) * 128], self.identb[:], reads=x_.b, writes=pt.b)
                            self.copy("act" if hf else "dve", xgT[:, hf * 8:(hf + 1) * 8, st * 128:(st + 1) * 128],
                                      pt[:].rearrange("p (a b) -> p a b", a=8), reads=pt.b, writes=[xgT.b[st // 4]])
                    for fg in range(F // FG):
                        hb = hid[fg % 2]
                        dts = []
                        for pr in range(FG // 4):
                            f0 = fg * FG + pr * 4
                            gt = wp[wi % NW]; wi += 1
                            ut = wp[wi % NW]; wi += 1
                            for (wt, W) in ((gt, Wg), (ut, Wu)):
                                src = W[:, f0 * 128:(f0 + 4) * 128].rearrange("(kc p) n -> p kc n", p=128)
                                S.dma("pool", wt[:].rearrange("p (kc n) -> p kc n", kc=KC), src, writes=wt.b)
                            for jj in range(4):
                                fl = pr * 4 + jj
                                for tt in range(2):
                                    tsl = slice(tt * 512, min((tt + 1) * 512, CAP))
                                    tw = tsl.stop - tsl.start
                                    p_g, p_u = pg[n % 2], pu[n % 2]
                                    s_ = sg[n % 2]
                                    n += 1
                                    for (pt_, wt) in ((p_g, gt), (p_u, ut)):
                                        for kc in range(KC):
                                            c0 = kc * 512 + jj * 128
                                            self.mm(pt_[:, 0:tw], wt[:, c0:c0 + 128], xgT[:, kc, tsl], kc == 0, kc == KC - 1,
                                                    reads=wt.b + [xgT.b[tt]], writes=pt_.b)
                                    self.act(s_[:, 0:tw], p_g[:, 0:tw], AF.Silu, reads=p_g.b, writes=s_.b)
                                    self.tt(hb[:, fl, tsl], p_u[:, 0:tw], s_[:, 0:tw], ALU.mult, reads=p_u.b + s_.b, writes=[hb.b[tt]])
                        for pr in range(FG // 4):
                            f0 = fg * FG + pr * 4
                            dt_ = wp[wi % NW]; wi += 1
                            src = Wd[f0 * 128:(f0 + 4) * 128, :].rearrange("(c p) n -> p c n", p=128)
                            S.dma("pool", dt_[:].rearrange("p (c n) -> p c n", c=4), src, writes=dt_.b)
                            dts.append(dt_)
                        for st in range(CAP // 128):
                            for q in range(4):
                                p = pd[nd % 3]
                                nd += 1
                                for fl in range(FG):
                                    d = dts[fl // 4]
                                    c0 = (fl % 4) * D + q * 512
                                    self.mm(p[:], hb[:, fl, st * 128:(st + 1) * 128], d[:, c0:c0 + 512], fl == 0, fl == FG - 1,
                                            reads=d.b + [hb.b[st // 4]], writes=p.b)
                                ab = [acc.b[st * 4 + q]]
                                dst = acc[:, st, q * 512:(q + 1) * 512]
                                if fg == 0:
                                    self.copy("dve", dst, p[:], reads=p.b, writes=ab)
                                else:
                                    self.tt(dst, p[:], dst, ALU.add, reads=p.b + ab, writes=ab)
                    S.dma("sp", YG[e_].rearrange("(st p) f -> p st f", p=128), acc[:], reads=acc.b, writes=[YGb[e_]])
            S.barrier()
            with contextlib.ExitStack() as es3:
                GfB = self.sb(es3, [128, D], F32)
                diag = [self.sb(es3, [128, 128], F32) for _ in range(2)]
                Gt = [self.sb(es3, [128, D], F32) for _ in range(3)]
                at = [self.sb(es3, [128, D], F32) for _ in range(2)]
                xTt = [self.sb(es3, [128, KC, 128], F32) for _ in range(2)]
                px = [self.ps(es3, [128, 512], F32) for _ in range(8)]
                for kc in range(KC):
                    dg = diag[kc % 2]
                    p = px[kc % 8]
                    self.ts(dg[:], self.ident[:], G[:, kc:kc + 1], None, ALU.mult, None, reads=(), writes=dg.b)
                    self.mm(p[:, 0:128], self.ones32[:], dg[:], True, True, reads=dg.b, writes=p.b)
                    self.copy("act", GfB[:, kc * 128:(kc + 1) * 128], p[:, 0:128], reads=p.b, writes=GfB.b)
                for g_ in Gt:
                    S.add("dve", lambda e, g_=g_: e.memset(g_[:], 0.0), writes=g_.b)
                ob = Buf()
                ng = 0
                for g in range(16):
                    x_ = xTt[g % 2]
                    a_ = at[g % 2]
                    S.dma("sp", x_[:], self.R[R_in][:, :, g * 128:(g + 1) * 128], reads=self.Rb[R_in], writes=x_.b)
                    pq = px[(g % 2) * 4:(g % 2) * 4 + 4]
                    for q in range(4):
                        for i in range(4):
                            kc = q * 4 + i
                            self.tr(pq[q][:, i * 128:(i + 1) * 128], x_[:, kc, :], self.ident[:], reads=x_.b, writes=pq[q].b)
                    for e_ in range(NE):
                        gt_ = Gt[ng % 3]
                        ng += 1
                        off = bass.IndirectOffsetOnAxis(ap=idx_all[:, g, e_:e_ + 1], axis=0)
                        S.add("pool", lambda eng, e_=e_, off=off, gt_=gt_: eng.indirect_dma_start(
                            out=gt_[:, :], out_offset=None, in_=YG[e_][:, :], in_offset=off,
                            bounds_check=self._bc_reg(eng, CAP - 1), oob_is_err=False),
                            reads=[YGb[e_]], writes=gt_.b, dma=True)
                        if e_ == 0:
                            self.ts(a_[:], gt_[:], wts_all[:, g, 0:1], None, ALU.mult, None, reads=gt_.b, writes=a_.b)
                        else:
                            self.stt(a_[:], gt_[:], wts_all[:, g, e_:e_ + 1], a_[:], ALU.mult, ALU.add, reads=gt_.b + a_.b, writes=a_.b)
                    self.tt(a_[:], a_[:], GfB[:], ALU.mult, reads=a_.b + GfB.b, writes=a_.b)
                    for q in range(4):
                        qs_ = slice(q * 512, (q + 1) * 512)
                        self.tt(a_[:, qs_], pq[q][:], a_[:, qs_], ALU.add, reads=pq[q].b + a_.b, writes=a_.b)
                    S.dma("sp", self.out[g * 128:(g + 1) * 128, :], a_[:], reads=a_.b, writes=[ob])

    def phase_attn(self, R_in, R_out):
        S, I = self.S, self.I
        qs = self.dscr("qs", [NH, 128, T], BF16)
        ks = self.dscr("ks", [NH, 128, T], BF16)
        qsb = [Buf() for _ in range(NH)]
        ksb = [Buf() for _ in range(NH)]
        with contextlib.ExitStack() as es:
            vtok = self.sb(es, [128, 16, D], BF16)
            selb = self.sb(es, [128, NH, 8, 8], F32)
            with contextlib.ExitStack() as es1:
                hT = self.sb(es1, [128, KC, T], BF16, nb=4)
                self.phase_prenorm(hT, False, R_in, None, self.Aprm[:, 1, 0, :], self.modT[:, 1, 0:16])
                S.barrier()
                self.attn_qkv(hT, vtok, selb, qs, ks, qsb, ksb)
                S.barrier()
            with contextlib.ExitStack() as es2:
                oT = self.sb(es2, [128, KC, T], BF16)
                self.attn_core(vtok, selb, qs, ks, qsb, ksb, oT)
                S.barrier()
                self.phase_outproj(oT, I["attn_out"], self.modT[:, 1, 32:48], R_in, R_out)

    def attn_qkv(self, hT, vtok, selb, qs, ks, qsb, ksb):
        S, I = self.S, self.I
        W = I["qkv_w"]
        with contextlib.ExitStack() as es:
            wp = [self.sb(es, [128, KC, 128], BF16) for _ in range(6)]
            rope = self.sb(es, [128, 2, T], F32)
            qkg = self.sb(es, [128, 4], F32)
            pmask = self.sb(es, [128, 8, 8], F32)
            NB = 2
            x32 = [self.sb(es, [128, 512], F32) for _ in range(NB)]
            rs = [self.sb(es, [128, 512], F32) for _ in range(NB)]
            xn = [self.sb(es, [128, 512], F32) for _ in range(NB)]
            xsw = [self.sb(es, [128, 512], F32) for _ in range(NB)]
            t2 = [self.sb(es, [128, 512], F32) for _ in range(NB)]
            xb = [self.sb(es, [128, 512], BF16) for _ in range(NB)]
            sqh = [self.sb(es, [128, 512], BF16) for _ in range(NB)]
            kmean = [self.sb(es, [128, NBLK], F32) for _ in range(2)]
            gm = self.sb(es, [128, 8, 8], F32)
            mx8 = self.sb(es, [128, 8], F32)
            sel = self.sb(es, [128, 8], F32)
            pp = [self.ps(es, [128, 512], F32) for _ in range(3)]
            ps_s = [self.ps(es, [128, 512], F32) for _ in range(2)]
            ps_v = [self.ps(es, [128, 128], F32) for _ in range(2)]
            ps_gate = self.ps(es, [128, 8, 8], F32)
            S.dma("sp", rope[:], I["ropeT"], writes=rope.b)
            S.dma("sp", qkg[:], I["qk_gain"], writes=qkg.b)
            S.dma("sp", pmask[:], I["pastmask"], writes=pmask.b)
            units = [(h, which, tt) for h in range(NH) for which in ("k", "q") for tt in range(4)]
            NU = len(units)
            st = {"nv": 0}

            def wts(h):
                base = (h % 2) * 3
                return wp[base], wp[base + 1], wp[base + 2]

            def P1(u):
                h, which, tt = units[u]
                wq, wk, wv = wts(h)
                if which == "k" and tt == 0:
                    for k_, w_ in enumerate((wq, wk, wv)):
                        self.wload(w_, W, k_ * D + h * 128, 128)
                w = wk if which == "k" else wq
                tsl = slice(tt * 512, (tt + 1) * 512)
                p = pp[u % 3]
                for kc in range(KC):
                    self.mm(p[:], w[:, kc, :], hT[:, kc, tsl], kc == 0, kc == KC - 1, reads=w.b + [hT.b[tt]], writes=p.b)
                vi = (0 if which == "k" else 4) + tt
                for tk in (2 * vi, 2 * vi + 1):
                    pv = ps_v[st["nv"] % 2]
                    st["nv"] += 1
                    for kc in range(KC):
                        self.mm(pv[:], hT[:, kc, tk * 128:(tk + 1) * 128], wv[:, kc, :], kc == 0, kc == KC - 1,
                                reads=[hT.b[tk // 4]] + wv.b, writes=pv.b)
                    self.copy("act" if tk % 2 else "dve", vtok[:, tk, h * 128:(h + 1) * 128], pv[:], reads=pv.b, writes=vtok.b)
                x_ = x32[u % NB]
                self.copy("dve", x_[:], p[:], reads=p.b, writes=x_.b)
                self.act(sqh[u % NB][:], x_[:], AF.Square, reads=x_.b, writes=sqh[u % NB].b)

            def P2(u):
                h, which, tt = units[u]
                gi = 1 if which == "k" else 0
                p2 = ps_s[u % 2]
                x_, r_, n_, w_ = x32[u % NB], rs[u % NB], xn[u % NB], xsw[u % NB]
                self.mm(p2[:], self.onesH[:], sqh[u % NB][:], True, True, reads=sqh[u % NB].b, writes=p2.b)
                self.act(r_[:], p2[:], AF.Ln, reads=p2.b, writes=r_.b, bias=self.epsT[:], scale=1.0)
                self.act(r_[:], r_[:], AF.Exp, reads=r_.b, writes=r_.b, scale=-0.5)
                self.stt(n_[:], x_[:], qkg[:, gi:gi + 1], r_[:], ALU.mult, ALU.mult, reads=x_.b + r_.b + qkg.b, writes=n_.b)
                S.dma("sp", w_[0:64, :], n_[64:128, :], reads=n_.b, writes=w_.b)
                S.dma("sp", w_[64:128, :], n_[0:64, :], reads=n_.b, writes=w_.b)

            def P3(u):
                h, which, tt = units[u]
                tsl = slice(tt * 512, (tt + 1) * 512)
                r_, n_, w_, t_, b_ = rs[u % NB], xn[u % NB], xsw[u % NB], t2[u % NB], xb[u % NB]
                kr = r_
                self.tt(t_[:], w_[:], rope[:, 1, tsl], ALU.mult, reads=w_.b + rope.b, writes=t_.b)
                self.tt(n_[:], n_[:], rope[:, 0, tsl], ALU.mult, reads=n_.b + rope.b, writes=n_.b)
                self.tt(kr[:], n_[:], t_[:], ALU.add, reads=n_.b + t_.b, writes=kr.b)
                self.copy("act", b_[:], kr[:], reads=kr.b, writes=b_.b)
                km = kmean[h % 2]
                if which == "k":
                    S.dma("sp", ks[h][:, tsl], b_[:], reads=b_.b, writes=[ksb[h]])
                    S.add("dve", lambda e: e.reduce_sum(km[:, 2 * tt:2 * tt + 2], kr[:].rearrange("p (n k) -> p n k", n=2), AX.X),
                          reads=kr.b, writes=km.b)
                    if tt == 3:
                        self.ts(km[:], km[:], 1.0 / BLK, None, ALU.mult, None, reads=km.b, writes=km.b)
                else:
                    S.dma("sp", qs[h][:, tsl], b_[:], reads=b_.b, writes=[qsb[h]])
                    if tt >= 2:
                        for i in range(4):
                            qi = (tt - 2) * 4 + i
                            self.mm(ps_gate[:, qi, :], kr[:, i * 128:(i + 1) * 128], km[:], True, True,
                                    reads=kr.b + km.b, writes=ps_gate.b)
                    if tt == 3:
                        self.tt(gm[:], ps_gate[:], pmask[:], ALU.add, reads=ps_gate.b + pmask.b, writes=gm.b)
                        for qi in range(8):
                            S.add("dve", lambda e, qi=qi: e.max(mx8[:], gm[:, qi, :]), reads=gm.b, writes=mx8.b)
                            self.ts(sel[:], gm[:, qi, :], mx8[:, 2:3], None, ALU.is_ge, None, reads=gm.b + mx8.b, writes=sel.b)
                            self.ts(selb[:, h, qi, :], sel[:], -1.0, -NEG, ALU.add, ALU.mult, reads=sel.b, writes=selb.b)

            for i in range(NU + 2):
                if i < NU:
                    P1(i)
                if 0 <= i - 1 < NU:
                    P2(i - 1)
                if 0 <= i - 2 < NU:
                    P3(i - 2)

    def attn_core(self, vtok, selb, qs, ks, qsb, ksb, oT):
        S, I = self.S, self.I
        with contextlib.ExitStack() as es:
            tri = self.sb(es, [128, 256], F32)
            qT = [self.sb(es, [128, T], BF16) for _ in range(2)]
            kT = [self.sb(es, [128, T], BF16) for _ in range(2)]
            P = [self.sb(es, [128, T], BF16) for _ in range(3)]
            PT = [self.sb(es, [128, T], BF16) for _ in range(2)]
            rsum = [self.sb(es, [128, 16], F32) for _ in range(3)]
            rtot = [self.sb(es, [128, 1], F32) for _ in range(2)]
            stri = [self.sb(es, [128, 256], F32) for _ in range(2)]
            otok = [self.sb(es, [128, 128], BF16) for _ in range(2)]
            ps_S = [self.ps(es, [128, 256], F32) for _ in range(2)]
            ps_PT = [self.ps(es, [128, 512], BF16) for _ in range(2)]
            ps_O = [self.ps(es, [128, 128], F32) for _ in range(2)]
            ps_OT = [self.ps(es, [128, 128], BF16) for _ in range(2)]
            S.dma("sp", tri[:], I["tri"], writes=tri.b)
            iters = [(h, qt) for h in range(NH) for qt in range(16)]
            N = len(iters)
            st = {"nS": 0, "nPT": 0}
            nkts = {}

            def stageA(it):
                h, qt = iters[it]
                q_, k_ = qT[h % 2], kT[h % 2]
                if qt == 0:
                    S.dma("sp", q_[:], qs[h], reads=[qsb[h]], writes=q_.b)
                    S.dma("sp", k_[:], ks[h], reads=[ksb[h]], writes=k_.b)
                own, half = qt // 2, qt % 2
                P_, rs_ = P[it % 3], rsum[it % 3]
                qsl = slice(qt * 128, (qt + 1) * 128)
                S.add("dve", lambda e: e.memset(rs_[:], 0.0), writes=rs_.b)
                col = 0
                nop = 0
                for nb in range(own):
                    p = ps_S[st["nS"] % 2]
                    st["nS"] += 1
                    self.mm(p[:], q_[:, qsl], k_[:, nb * 256:(nb + 1) * 256], True, True, reads=q_.b + k_.b, writes=p.b)
                    bias = selb[:, h, qt - 8, nb:nb + 1] if own >= 4 else None
                    self.act(P_[:, col:col + 256], p[:], AF.Exp, reads=p.b + selb.b, writes=P_.b + rs_.b, bias=bias, scale=ATT_SCALE,
                             accum_out=rs_[:, nop:nop + 1])
                    col += 256
                    nop += 1
                nk = 128 if half == 0 else 256
                p = ps_S[st["nS"] % 2]
                st_ = stri[st["nS"] % 2]
                st["nS"] += 1
                self.mm(p[:, 0:nk], q_[:, qsl], k_[:, own * 256:own * 256 + nk], True, True, reads=q_.b + k_.b, writes=p.b)
                msk = tri[:, 128:256] if half == 0 else tri[:, 0:256]
                self.tt(st_[:, 0:nk], p[:, 0:nk], msk, ALU.add, reads=p.b + tri.b, writes=st_.b)
                self.act(P_[:, col:col + nk], st_[:, 0:nk], AF.Exp, reads=st_.b, writes=P_.b + rs_.b, scale=ATT_SCALE,
                         accum_out=rs_[:, nop:nop + 1])
                col += nk
                nkts[it] = col // 128

            def stageB(it):
                P_, PT_ = P[it % 3], PT[it % 2]
                nkt = nkts[it]
                for g0 in range(0, nkt, 4):
                    pp_ = ps_PT[st["nPT"] % 2]
                    st["nPT"] += 1
                    gn = min(4, nkt - g0)
                    for i in range(gn):
                        kt = g0 + i
                        self.tr(pp_[:, i * 128:(i + 1) * 128], P_[:, kt * 128:(kt + 1) * 128], self.identb[:], reads=P_.b, writes=pp_.b)
                    self.copy("dve", PT_[:, g0 * 128:(g0 + gn) * 128], pp_[:, 0:gn * 128], reads=pp_.b, writes=PT_.b)

            def stageC(it):
                h, qt = iters[it]
                PT_, rs_, rt_ = PT[it % 2], rsum[it % 3], rtot[it % 2]
                ot_, pO, pOT = otok[it % 2], ps_O[it % 2], ps_OT[it % 2]
                nkt = nkts[it]
                qsl = slice(qt * 128, (qt + 1) * 128)
                for kt in range(nkt):
                    self.mm(pO[:], PT_[:, kt * 128:(kt + 1) * 128], vtok[:, kt, h * 128:(h + 1) * 128], kt == 0, kt == nkt - 1,
                            reads=PT_.b + vtok.b, writes=pO.b)
                S.add("dve", lambda e: e.reduce_sum(rt_[:], rs_[:], AX.X), reads=rs_.b, writes=rt_.b)
                S.add("dve", lambda e: e.reciprocal(rt_[:], rt_[:]), reads=rt_.b, writes=rt_.b)
                self.act(ot_[:], pO[:], AF.Identity, reads=pO.b + rt_.b, writes=ot_.b, scale=rt_[:, 0:1])
                self.tr(pOT[:], ot_[:], self.identb[:], reads=ot_.b, writes=pOT.b)
                self.copy("dve", oT[:, h, qsl], pOT[:], reads=pOT.b, writes=oT.b)

            for i in range(N + 2):
                if i < N:
                    stageA(i)
                if 0 <= i - 1 < N:
                    stageB(i - 1)
                if 0 <= i - 2 < N:
                    stageC(i - 2)

    def phase_final(self, cur):
        S = self.S
        with contextlib.ExitStack() as es:
            xT = [self.sb(es, [128, KC, 512], F32) for _ in range(2)]
            xo = [self.sb(es, [128, D], F32) for _ in range(2)]
            ps_tr = [self.ps(es, [128, 512], F32) for _ in range(4)]
            n = 0
            no = 0
            ob = Buf()
            for tt in range(4):
                x_ = xT[tt % 2]
                S.dma("sp", x_[:], self.R[cur][:, :, tt * 512:(tt + 1) * 512], reads=self.Rb[cur], writes=x_.b)
                for s in range(4):
                    o_ = xo[no % 2]
                    no += 1
                    for c4 in range(4):
                        p = ps_tr[n % 4]
                        n += 1
                        for i in range(4):
                            kc = c4 * 4 + i
                            self.tr(p[:, i * 128:(i + 1) * 128], x_[:, kc, s * 128:(s + 1) * 128], self.ident[:], reads=x_.b, writes=p.b)
                        self.copy("act" if c4 % 2 else "dve", o_[:, c4 * 512:(c4 + 1) * 512], p[:], reads=p.b, writes=o_.b)
                    r0 = tt * 512 + s * 128
                    S.dma("sp", self.out[r0:r0 + 128, :], o_[:], reads=o_.b, writes=[ob])


def _pk(v):
    v = np.asarray(v, dtype=np.float32)
    return np.ascontiguousarray(v.reshape(-1, 128).T)


def _consts():
    half = HD // 2
    inv_freq = np.exp(-math.log(10000.0) * np.arange(half, dtype=np.float32) / half).astype(np.float32)
    ang = np.arange(T, dtype=np.float32)[:, None] * inv_freq[None, :]
    cos = np.cos(ang).astype(np.float32).T
    sin = np.sin(ang).astype(np.float32).T
    ropeT = np.zeros((128, 2, T), np.float32)
    ropeT[:half, 0] = cos
    ropeT[half:, 0] = cos
    ropeT[:half, 1] = -sin
    ropeT[half:, 1] = sin
    tri = np.zeros((128, 256), np.float32)
    qi = np.arange(128)[:, None]
    ki = np.arange(128)[None, :]
    tri[:, 128:] = np.where(ki <= qi, 0.0, -1e9).astype(np.float32)
    pm = np.zeros((128, 8, 8), np.float32)
    for i in range(8):
        own = (8 + i) // 2
        pm[:, i, own:] = -1e30
    return ropeT, tri, pm


def make_in_map(inputs, b, stop=None):
    ropeT, tri, pm = _consts()
    m = {}
    m["x"] = np.ascontiguousarray(inputs["x"][b])
    m["cT"] = _pk(inputs["c"][b])
    m["mod_w"] = np.asarray(inputs["mod_w"])
    mb = np.asarray(inputs["mod_b"], np.float32)
    m["mod_bT"] = np.ascontiguousarray(np.stack([_pk(mb[0]), _pk(mb[1])], axis=1))
    g = [inputs["norm_mix"][0], inputs["norm_ffn"][0], inputs["norm_mix"][1], inputs["norm_ffn"][1]]
    m["gains"] = np.ascontiguousarray(np.stack([_pk(v) for v in g], axis=1))
    m["ident"] = np.eye(128, dtype=np.float32)
    m["conv_in"] = np.asarray(inputs["conv_in"][0])
    cw = np.asarray(inputs["conv_w"][0], np.float32)
    m["conv_wT"] = np.ascontiguousarray(np.stack([_pk(cw[k]) for k in range(3)], axis=1))
    m["conv_out"] = np.asarray(inputs["conv_out"][0])
    m["ffn_gate"] = np.asarray(inputs["ffn_gate"][0])
    m["ffn_up"] = np.asarray(inputs["ffn_up"][0])
    m["ffn_down"] = np.asarray(inputs["ffn_down"][0])
    if stop is None or stop >= 3:
        m["qkv_w"] = np.asarray(inputs["qkv_w"][0])
        gq = np.asarray(inputs["q_norm"][0], np.float32)
        gk = np.asarray(inputs["k_norm"][0], np.float32)
        m["qk_gain"] = np.ascontiguousarray(np.stack([gq, gk, np.roll(gq, 64), np.roll(gk, 64)], axis=1))
        m["ropeT"] = ropeT
        m["tri"] = tri
        m["pastmask"] = pm
        m["attn_out"] = np.asarray(inputs["attn_out"][0])
    if stop is None or stop >= 4:
        rw = np.asarray(inputs["router_w"][0], np.float32)
        m["router_w"] = np.ascontiguousarray(rw.reshape(KC, 128, NE).transpose(1, 0, 2))
        m["router_b"] = np.ascontiguousarray(np.broadcast_to(np.asarray(inputs["router_b"][0], np.float32)[None, :], (128, NE)))
        m["ustrict"] = np.ascontiguousarray(np.triu(np.ones((128, 128), np.float32), 1))
        m["exp_gate"] = np.asarray(inputs["exp_gate"][0])
        m["exp_up"] = np.asarray(inputs["exp_up"][0])
        m["exp_down"] = np.asarray(inputs["exp_down"][0])
    return m


_CACHE = {}


def kernel(**inputs):
    if "nc" not in _CACHE:
        _CACHE["nc"] = Prog().build()
    nc = _CACHE["nc"]
    n = 8
    in_maps = [make_in_map(inputs, b) for b in range(n)]
    res = run_bass_kernel_spmd(nc, in_maps, core_ids=list(range(n)))
    return np.stack([np.asarray(r["out"], dtype=np.float32) for r in res.results], axis=0)
```

```python
import contextlib
import math
import numpy as np
import concourse.bass as bass
import concourse.mybir as mybir
from concourse.bass_utils import run_bass_kernel_spmd

F32 = mybir.dt.float32
BF16 = mybir.dt.bfloat16
AF = mybir.ActivationFunctionType
ALU = mybir.AluOpType
AX = mybir.AxisListType

T = 2048
D = 2048
KC = 16
NH = 16
HD = 128
BLK = 256
NBLK = 8
F_DENSE = 5632
F_EXP = 7168
NE = 8
EPS = 1e-6
ATT_SCALE = HD ** -0.5
NEG = -30000.0

ENGINES = ("pe", "act", "dve", "pool", "sp")
SIG_LIMIT = 20000
N_DMA_SEMS = {"sp": 24, "act": 4, "pool": 12}


class Buf:
    __slots__ = ("last_w", "readers")

    def __init__(self):
        self.last_w = None
        self.readers = {}


class Op:
    __slots__ = ("idx", "eng", "fn", "is_dma", "deps", "needs_sig", "sig", "dma_sem", "dma_val", "dma_prev")

    def __init__(self, idx, eng, fn, is_dma):
        self.idx = idx
        self.eng = eng
        self.fn = fn
        self.is_dma = is_dma
        self.deps = {}
        self.needs_sig = False
        self.sig = None
        self.dma_sem = None
        self.dma_val = 0
        self.dma_prev = None


class Sched:
    def __init__(self, nc):
        self.nc = nc
        self.ops = []
        self.last_compute = {}
        self.out_dmas = []

    def add(self, eng, fn, reads=(), writes=(), dma=False):
        op = Op(len(self.ops), eng, fn, dma)
        deps = op.deps

        def dep(o, raw):
            if o is None:
                return
            key = (o.idx if o.is_dma else o.eng, raw)
            cur = deps.get(key)
            if cur is None or cur.idx < o.idx:
                deps[key] = o

        for b in reads:
            dep(b.last_w, True)
        for b in writes:
            dep(b.last_w, True)
            for r in b.readers.values():
                dep(r, False)
        for b in reads:
            b.readers[("d", op.idx) if dma else eng] = op
        for b in writes:
            b.last_w = op
            b.readers = {}
        self.ops.append(op)
        if dma:
            self.out_dmas.append(op)
        else:
            self.last_compute[eng] = op
        return op

    def dma(self, q, out, in_, reads=(), writes=()):
        return self.add(q, lambda e: e.dma_start(out=out, in_=in_), reads, writes, dma=True)

    def barrier(self):
        last = dict(self.last_compute)
        dmas = list(self.out_dmas)
        for e in ENGINES:
            op = Op(len(self.ops), e, None, False)
            for e2, o in last.items():
                if e2 != e:
                    op.deps[(e2, True)] = o
            for o in dmas:
                op.deps[(o.idx, True)] = o
            self.ops.append(op)
        self.out_dmas = []

    def emit(self):
        nc = self.nc
        self.barrier()
        ops = self.ops
        for op in ops:
            for ((_k, raw), o) in op.deps.items():
                if o.is_dma:
                    continue
                if o.eng == op.eng and not op.is_dma and (o.eng == "pe" or not raw):
                    continue
                o.needs_sig = True
        sig_cnt = {e: 0 for e in ENGINES}
        dma_cnt = {q: 0 for q in N_DMA_SEMS}
        dma_last = {}
        for op in ops:
            if op.is_dma:
                q = op.eng
                slot = dma_cnt[q] % N_DMA_SEMS[q]
                dma_cnt[q] += 1
                key = (q, slot)
                prev = dma_last.get(key)
                op.dma_prev = prev
                op.dma_sem = f"d_{q}_{slot}"
                op.dma_val = (prev.dma_val if prev else 0) + 16
                dma_last[key] = op
            elif op.needs_sig:
                sig_cnt[op.eng] += 1
                op.sig = sig_cnt[op.eng]
        sems = {}
        ctx = []

        def getsem(name):
            if name not in sems:
                g = nc.semaphore(name)
                sems[name] = g.__enter__()
                ctx.append(g)
            return sems[name]

        for e in ENGINES:
            for k in range((sig_cnt[e] + SIG_LIMIT - 1) // SIG_LIMIT):
                getsem(f"s_{e}_{k}")
        for q, n in N_DMA_SEMS.items():
            for s in range(min(n, dma_cnt[q])):
                getsem(f"d_{q}_{s}")
        per_eng = {e: [] for e in ENGINES}
        for op in ops:
            per_eng[op.eng].append(op)

        def run_engine(ename, eng):
            waited = {}

            def wait(nm, val):
                if waited.get(nm, 0) >= val:
                    return
                waited[nm] = val
                eng.wait_ge(sems[nm], val)

            for op in per_eng[ename]:
                for ((_k, raw), o) in op.deps.items():
                    if o.is_dma:
                        wait(o.dma_sem, o.dma_val)
                    else:
                        if o.eng == op.eng and not op.is_dma and (o.eng == "pe" or not raw):
                            continue
                        k = (o.sig - 1) // SIG_LIMIT
                        wait(f"s_{o.eng}_{k}", o.sig - k * SIG_LIMIT)
                if op.is_dma and op.dma_prev is not None:
                    wait(op.dma_sem, op.dma_prev.dma_val)
                if op.fn is None:
                    continue
                ins = op.fn(eng)
                if op.is_dma:
                    ins.then_inc(sems[op.dma_sem], 16)
                elif op.sig is not None:
                    k = (op.sig - 1) // SIG_LIMIT
                    ins.then_inc(sems[f"s_{op.eng}_{k}"], 1)

        with nc.Block() as block:
            @block.tensor
            def _(e):
                run_engine("pe", e)

            @block.scalar
            def _(e):
                run_engine("act", e)

            @block.vector
            def _(e):
                run_engine("dve", e)

            @block.gpsimd
            def _(e):
                run_engine("pool", e)

            @block.sync
            def _(e):
                run_engine("sp", e)
        for g in reversed(ctx):
            g.__exit__(None, None, None)
        return {e: len(per_eng[e]) for e in ENGINES}


class TB:
    def __init__(self, t, nb=1):
        self.t = t
        self.b = [Buf() for _ in range(nb)]

    def __getitem__(self, k):
        return self.t[k]


class _HTView:
    def __init__(self, tb):
        self.tb = tb
        self.b = _AnyIdx(tb.b[0])

    def __getitem__(self, k):
        p, kc, tsl = k
        return self.tb.t[p, kc, 0:512]


class _AnyIdx:
    def __init__(self, b):
        self._b = b

    def __getitem__(self, i):
        return self._b

class Prog:
    def __init__(self, stop_after=None):
        self.stop_after = stop_after
        self.nc = bass.Bass("TRN2", target_bir_lowering=False)
        self.S = Sched(self.nc)
        self.uid = 0

    def din(self, name, shape, dt=F32):
        return self.nc.dram_tensor(name, list(shape), dt, kind="ExternalInput").ap()

    def dscr(self, name, shape, dt=F32):
        return self.nc.dram_tensor(name, list(shape), dt, kind="Internal").ap()

    def sb(self, es, shape, dt, nb=1):
        self.uid += 1
        return TB(es.enter_context(self.nc.sbuf_tensor(f"sb{self.uid}", list(shape), dt)), nb)

    def ps(self, es, shape, dt=F32, nb=1):
        self.uid += 1
        ncol = 512 if dt == F32 else 1024
        full = es.enter_context(self.nc.psum_tensor(f"ps{self.uid}", [128, ncol], dt))
        n = 1
        for d in shape[1:]:
            n *= d
        v = full[:, 0:n]
        if len(shape) == 3:
            v = v.rearrange("p (a b) -> p a b", a=shape[1])
        return TB(v, nb)

    def _bc_reg(self, eng, val):
        if getattr(self, "_bcr", None) is None:
            self._bcr = eng.to_reg(val)
        return self._bcr

    def mm(self, out, lhsT, rhs, start, stop, reads, writes):
        self.S.add("pe", lambda e: e.matmul(out, lhsT, rhs, start=start, stop=stop), reads, writes)

    def tr(self, out, in_, ident, reads, writes):
        self.S.add("pe", lambda e: e.transpose(out, in_, ident), reads, writes)

    def act(self, out, in_, func, reads, writes, bias=None, scale=None, accum_out=None):
        kw = {}
        if bias is not None:
            kw["bias"] = bias
        if scale is not None:
            kw["scale"] = scale
        if accum_out is not None:
            kw["accum_out"] = accum_out
        self.S.add("act", lambda e: e.activation(out, in_, func, **kw), reads, writes)

    def copy(self, eng, out, in_, reads, writes):
        if eng == "act":
            self.S.add("act", lambda e: e.copy(out, in_), reads, writes)
        else:
            self.S.add(eng, lambda e: e.tensor_copy(out, in_), reads, writes)

    def tt(self, out, in0, in1, op, reads, writes, eng="dve"):
        self.S.add(eng, lambda e: e.tensor_tensor(out, in0, in1, op), reads, writes)

    def ts(self, out, in0, s1, s2, op0, op1, reads, writes, eng="dve"):
        if op1 is None:
            self.S.add(eng, lambda e: e.tensor_scalar(out, in0, s1, None, op0), reads, writes)
        else:
            self.S.add(eng, lambda e: e.tensor_scalar(out, in0, s1, s2, op0, op1), reads, writes)

    def stt(self, out, in0, scalar, in1, op0, op1, reads, writes, eng="dve"):
        self.S.add(eng, lambda e: e.scalar_tensor_tensor(out, in0, scalar, in1, op0, op1), reads, writes)

    def build(self):
        nc, S = self.nc, self.S
        stop = self.stop_after
        I = {}
        I["x"] = self.din("x", [T, D])
        I["cT"] = self.din("cT", [128, KC])
        I["mod_w"] = self.din("mod_w", [2, D, 6 * D])
        I["mod_bT"] = self.din("mod_bT", [128, 2, 96])
        I["gains"] = self.din("gains", [128, 4, KC])
        I["ident"] = self.din("ident", [128, 128])
        I["conv_in"] = self.din("conv_in", [D, 3 * D])
        I["conv_wT"] = self.din("conv_wT", [128, 3, KC])
        I["conv_out"] = self.din("conv_out", [D, D])
        I["ffn_gate"] = self.din("ffn_gate", [D, F_DENSE])
        I["ffn_up"] = self.din("ffn_up", [D, F_DENSE])
        I["ffn_down"] = self.din("ffn_down", [F_DENSE, D])
        if stop is None or stop >= 3:
            I["qkv_w"] = self.din("qkv_w", [D, 3 * D])
            I["qk_gain"] = self.din("qk_gain", [128, 4])
            I["ropeT"] = self.din("ropeT", [128, 2, T])
            I["tri"] = self.din("tri", [128, 256])
            I["pastmask"] = self.din("pastmask", [128, 8, 8])
            I["attn_out"] = self.din("attn_out", [D, D])
        if stop is None or stop >= 4:
            I["router_w"] = self.din("router_w", [128, KC, NE])
            I["router_b"] = self.din("router_b", [128, NE])
            I["ustrict"] = self.din("ustrict", [128, 128])
            I["exp_gate"] = self.din("exp_gate", [NE, D, F_EXP])
            I["exp_up"] = self.din("exp_up", [NE, D, F_EXP])
            I["exp_down"] = self.din("exp_down", [NE, F_EXP, D])
        self.I = I
        self.out = nc.dram_tensor("out", [T, D], F32, kind="ExternalOutput").ap()
        self.R = [self.dscr("R0", [128, KC, T]), self.dscr("R1", [128, KC, T])]
        self.Rb = [[Buf() for _ in range(KC)], [Buf() for _ in range(KC)]]

        with contextlib.ExitStack() as top:
            self.top = top
            self.ident = self.sb(top, [128, 128], F32)
            self.identb = self.sb(top, [128, 128], BF16)
            self.onesD = self.sb(top, [128, 128], BF16)
            self.onesH = self.sb(top, [128, 128], BF16)
            self.ones32 = self.sb(top, [128, 128], F32)
            self.epsT = self.sb(top, [128, 1], F32)
            self.modT = self.sb(top, [128, 2, 96], F32)
            self.Aprm = self.sb(top, [128, 2, 2, KC], F32)
            self.gains = self.sb(top, [128, 4, KC], F32)
            self.convw = self.sb(top, [128, 3, KC], F32)
            self.phase_setup()
            S.barrier()
            self.phase_mod()
            S.barrier()
            with contextlib.ExitStack() as es:
                hT = self.sb(es, [128, KC, T], BF16, nb=4)
                self.phase_prenorm(hT, True, None, 0, self.Aprm[:, 0, 0, :], self.modT[:, 0, 0:16])
                S.barrier()
                cur = 0
                if stop is None or stop >= 1:
                    zT = self.sb(es, [128, KC, T], BF16, nb=1)
                    self.phase_conv(hT, zT)
                    S.barrier()
                    cur = 1
            if stop is None or stop >= 2:
                self.phase_ffn(R_in=1, R_out=0, A=self.Aprm[:, 0, 1, :], B=self.modT[:, 0, 48:64], G=self.modT[:, 0, 80:96],
                               experts=[(I["ffn_gate"], I["ffn_up"], I["ffn_down"], F_DENSE // 128)], FG=4, moe=False)
                S.barrier()
                cur = 0
            if stop is None or stop >= 3:
                self.phase_attn(R_in=0, R_out=1)
                S.barrier()
                cur = 1
            if stop is None or stop >= 4:
                ex = [(I["exp_gate"][e], I["exp_up"][e], I["exp_down"][e], F_EXP // 128) for e in range(NE)]
                self.phase_moe_sparse(R_in=1, A=self.Aprm[:, 1, 1, :], B=self.modT[:, 1, 48:64], G=self.modT[:, 1, 80:96], experts=ex)
            else:
                self.phase_final(cur)
            counts = S.emit()
        self.counts = counts
        return nc

    def phase_setup(self):
        S, I = self.S, self.I
        b = Buf()
        S.dma("sp", self.ident[:], I["ident"], writes=[b])
        S.dma("sp", self.gains[:], I["gains"], writes=[Buf()])
        S.dma("sp", self.convw[:], I["conv_wT"], writes=[Buf()])
        S.add("dve", lambda e: e.tensor_copy(self.identb[:], self.ident[:]), reads=[b], writes=[Buf()])
        S.add("dve", lambda e: e.memset(self.onesD[:], 1.0 / D), writes=[Buf()])
        S.add("dve", lambda e: e.memset(self.onesH[:], 1.0 / HD), writes=[Buf()])
        S.add("dve", lambda e: e.memset(self.ones32[:], 1.0), writes=[Buf()])
        S.add("dve", lambda e: e.memset(self.epsT[:], EPS), writes=[Buf()])

    def phase_mod(self):
        S, I = self.S, self.I
        with contextlib.ExitStack() as es:
            c_sb = self.sb(es, [128, KC], F32)
            cact = self.sb(es, [128, KC], BF16)
            mb = self.sb(es, [128, 2, 96], F32)
            wts = [self.sb(es, [128, KC, 512], BF16) for _ in range(4)]
            pm = [self.ps(es, [128, 96], F32) for _ in range(2)]
            S.dma("sp", c_sb[:], I["cT"], writes=c_sb.b)
            S.dma("sp", mb[:], I["mod_bT"], writes=mb.b)
            self.act(cact[:], c_sb[:], AF.Silu, reads=c_sb.b, writes=cact.b)
            n = 0
            for i in range(2):
                for cb in range(24):
                    w = wts[n % 4]
                    n += 1
                    src = I["mod_w"][i][:, cb * 512:(cb + 1) * 512].rearrange("(kc p) n -> p kc n", p=128)
                    S.dma("pool", w[:], src, writes=w.b)
                    for jj in range(4):
                        j = cb * 4 + jj
                        for kc in range(KC):
                            self.mm(pm[i][:, j:j + 1], w[:, kc, jj * 128:(jj + 1) * 128], cact[:, kc:kc + 1],
                                    kc == 0, kc == KC - 1, reads=w.b + cact.b, writes=pm[i].b)
                self.tt(self.modT[:, i, :], pm[i][:], mb[:, i, :], ALU.add, reads=pm[i].b + mb.b, writes=self.modT.b)
                for k, (sc_lo, gi) in enumerate(((16, 2 * i), (64, 2 * i + 1))):
                    self.stt(self.Aprm[:, i, k, :], self.modT[:, i, sc_lo:sc_lo + 16], 1.0, self.gains[:, gi, :],
                             ALU.add, ALU.mult, reads=self.modT.b, writes=self.Aprm.b)

    def norm_tile(self, xT, A, B, out_ap_fn, out_bufs, sq, rstd, tmps, ps_ss, h32_cb=None):
        self.act(sq[:], xT[:], AF.Square, reads=xT.b, writes=sq.b)
        for kc in range(KC):
            self.mm(ps_ss[:], self.onesD[:], sq[:, kc, :], kc == 0, kc == KC - 1, reads=sq.b, writes=ps_ss.b)
        self.act(rstd[:], ps_ss[:], AF.Ln, reads=ps_ss.b, writes=rstd.b, bias=self.epsT[:], scale=1.0)
        self.act(rstd[:], rstd[:], AF.Exp, reads=rstd.b, writes=rstd.b, scale=-0.5)
        for kc in range(KC):
            tmp = tmps[kc % len(tmps)]
            self.stt(tmp[:], xT[:, kc, :], A[:, kc:kc + 1], rstd[:], ALU.mult, ALU.mult, reads=xT.b + rstd.b, writes=tmp.b)
            if h32_cb is None:
                self.act(out_ap_fn(kc), tmp[:], AF.Identity, reads=tmp.b, writes=out_bufs, bias=B[:, kc:kc + 1], scale=1.0)
            else:
                h32_cb(kc, tmp, B[:, kc:kc + 1], out_ap_fn(kc), out_bufs)

    def phase_prenorm(self, hT, src_tokmajor, R_src, R_dst, A, B, t_lo=0, n_tt=4, h32_cb=None, tt_done_cb=None, nbuf=2):
        S, I = self.S, self.I
        with contextlib.ExitStack() as es:
            xTs = [self.sb(es, [128, KC, 512], F32) for _ in range(nbuf)]
            sqs = [self.sb(es, [128, KC, 512], BF16) for _ in range(nbuf)]
            rstds = [self.sb(es, [128, 512], F32) for _ in range(nbuf)]
            tmps = [self.sb(es, [128, 512], F32) for _ in range(2)]
            ps_ss = self.ps(es, [128, 512], F32)
            if src_tokmajor:
                xin = self.sb(es, [128, 4, D], F32)
                ps_tr = [self.ps(es, [128, 512], F32) for _ in range(2)]
            for ti in range(n_tt):
                tt = t_lo + ti
                tsl = slice(tt * 512, (tt + 1) * 512)
                xT, sq, rstd = xTs[ti % nbuf], sqs[ti % nbuf], rstds[ti % nbuf]
                if src_tokmajor:
                    S.dma("sp", xin[:], I["x"][tsl, :].rearrange("(s p) d -> p s d", p=128), writes=xin.b)
                    for kc in range(KC):
                        p = ps_tr[kc % 2]
                        for s in range(4):
                            self.tr(p[:, s * 128:(s + 1) * 128], xin[:, s, kc * 128:(kc + 1) * 128], self.ident[:], reads=xin.b, writes=p.b)
                        self.copy("act" if kc % 2 else "dve", xT[:, kc, :], p[:], reads=p.b, writes=xT.b)
                    S.dma("sp", self.R[R_dst][:, :, tsl], xT[:], reads=xT.b, writes=self.Rb[R_dst])
                else:
                    S.dma("sp", xT[:], self.R[R_src][:, :, tsl], reads=self.Rb[R_src], writes=xT.b)
                lt = ti
                self.norm_tile(xT, A, B, lambda kc: hT[:, kc, lt * 512:(lt + 1) * 512], [hT.b[lt]], sq, rstd, tmps, ps_ss, h32_cb=h32_cb)
                if tt_done_cb is not None:
                    tt_done_cb(ti)

    def wload(self, wt, src2d, c0, ncols):
        src = src2d[:, c0:c0 + ncols].rearrange("(kc p) n -> p kc n", p=128)
        self.S.dma("pool", wt[:, :, 0:ncols], src, writes=wt.b)

    def phase_conv(self, hT, zT):
        S, I = self.S, self.I
        Gm = self.modT[:, 0, 32:48]
        with contextlib.ExitStack() as es:
            wp = [self.sb(es, [128, KC, 128], BF16) for _ in range(6)]
            u = self.sb(es, [128, T + 2], F32)
            bsb = self.sb(es, [128, T], F32)
            t0 = self.sb(es, [128, T], F32)
            csb = [self.sb(es, [128, 512], F32) for _ in range(2)]
            pss = [self.ps(es, [128, 512], F32) for _ in range(6)]
            S.add("dve", lambda e: e.memset(u[:, 0:2], 0.0), writes=u.b)
            n = 0
            for g in range(8):
                for jj in range(2):
                    j = 2 * g + jj
                    wb_, wc_, wv_ = wp[(j % 2) * 3:(j % 2) * 3 + 3]
                    for k, w_ in enumerate((wb_, wc_, wv_)):
                        self.wload(w_, I["conv_in"], k * D + j * 128, 128)
                    cs = slice(0, 128)
                    for tt in range(4):
                        tsl = slice(tt * 512, (tt + 1) * 512)
                        pc, pv, pb = pss[(n % 2) * 3:(n % 2) * 3 + 3]
                        n += 1
                        for (pt, w) in ((pc, wc_), (pv, wv_), (pb, wb_)):
                            for kc in range(KC):
                                self.mm(pt[:], w[:, kc, cs], hT[:, kc, tsl], kc == 0, kc == KC - 1, reads=w.b + [hT.b[tt]], writes=pt.b)
                        c_ = csb[tt % 2]
                        self.copy("act", c_[:], pc[:], reads=pc.b, writes=c_.b)
                        self.tt(u[:, 2 + tt * 512:2 + (tt + 1) * 512], pv[:], c_[:], ALU.mult, reads=pv.b + c_.b, writes=u.b)
                        self.copy("act", bsb[:, tsl], pb[:], reads=pb.b, writes=bsb.b)
                    cw = self.convw
                    self.ts(t0[:], u[:, 2:T + 2], cw[:, 2, j:j + 1], None, ALU.mult, None, reads=u.b, writes=t0.b)
                    self.stt(t0[:], u[:, 1:T + 1], cw[:, 1, j:j + 1], t0[:], ALU.mult, ALU.add, reads=u.b + t0.b, writes=t0.b)
                    self.stt(t0[:], u[:, 0:T], cw[:, 0, j:j + 1], t0[:], ALU.mult, ALU.add, reads=u.b + t0.b, writes=t0.b)
                    self.tt(zT[:, j, :], bsb[:], t0[:], ALU.mult, reads=bsb.b + t0.b, writes=zT.b)
        S.barrier()
        self.phase_outproj(zT, I["conv_out"], Gm, R_in=0, R_out=1)

    def phase_outproj(self, zT, W, G, R_in, R_out):
        S = self.S
        with contextlib.ExitStack() as es:
            wp = [self.sb(es, [128, KC, 256], BF16) for _ in range(3)]
            xres = [self.sb(es, [128, T], F32) for _ in range(2)]
            xnew = [self.sb(es, [128, T], F32) for _ in range(2)]
            pss = [self.ps(es, [128, 512], F32) for _ in range(4)]
            n = 0
            for g in range(8):
                w = wp[g % 3]
                self.wload(w, W, g * 256, 256)
                for jj in range(2):
                    m = 2 * g + jj
                    xr, xn = xres[m % 2], xnew[m % 2]
                    S.dma("sp", xr[:], self.R[R_in][:, m, :], reads=[self.Rb[R_in][m]], writes=xr.b)
                    for tt in range(4):
                        tsl = slice(tt * 512, (tt + 1) * 512)
                        p = pss[n % 4]
                        n += 1
                        for kc in range(KC):
                            self.mm(p[:], w[:, kc, jj * 128:(jj + 1) * 128], zT[:, kc, tsl], kc == 0, kc == KC - 1, reads=w.b + zT.b, writes=p.b)
                        self.stt(xn[:, tsl], p[:], G[:, m:m + 1], xr[:, tsl], ALU.mult, ALU.add, reads=p.b + xr.b, writes=xn.b)
                    S.dma("sp", self.R[R_out][:, m, :], xn[:], reads=xn.b, writes=[self.Rb[R_out][m]])

    def phase_ffn(self, R_in, R_out, A, B, G, experts, FG, moe):
        S, I = self.S, self.I
        TG = 1024
        for half in range(2):
            with contextlib.ExitStack() as es:
                hTh = self.sb(es, [128, KC, TG], BF16, nb=2)
                wB = self.sb(es, [128, NE, TG], BF16, nb=2) if moe else None
                if moe:
                    self.moe_norm_route(hTh, wB, R_in, A, B, half)
                else:
                    self.phase_prenorm(hTh, False, R_in, None, A, B, t_lo=half * 2, n_tt=2)
                S.barrier()
                with contextlib.ExitStack() as es2:
                    acc = self.sb(es2, [128, KC, TG], F32, nb=KC * 2)
                    hid = [self.sb(es2, [128, FG, TG], BF16, nb=2) for _ in range(2)]
                    wp = [self.sb(es2, [128, KC * 256], BF16) for _ in range(4)]
                    sg = [self.sb(es2, [128, 512], F32) for _ in range(2)]
                    xres = [self.sb(es2, [128, TG], F32) for _ in range(2)]
                    xnew = [self.sb(es2, [128, TG], F32) for _ in range(2)]
                    pg = [self.ps(es2, [128, 512], F32) for _ in range(2)]
                    pu = [self.ps(es2, [128, 512], F32) for _ in range(2)]
                    pd = [self.ps(es2, [128, 512], F32) for _ in range(4)]
                    wi = 0
                    n = 0
                    nd = 0
                    first = True
                    for e, (Wg, Wu, Wd, F) in enumerate(experts):
                        for fg in range(F // FG):
                            hb = hid[fg % 2]
                            dts = []
                            for pr in range(FG // 2):
                                f0 = fg * FG + pr * 2
                                gt = wp[wi % 4]; wi += 1
                                ut = wp[wi % 4]; wi += 1
                                for (wt, W) in ((gt, Wg), (ut, Wu)):
                                    src = W[:, f0 * 128:(f0 + 2) * 128].rearrange("(kc p) n -> p kc n", p=128)
                                    S.dma("pool", wt[:].rearrange("p (kc n) -> p kc n", kc=KC), src, writes=wt.b)
                                for jj in range(2):
                                    fl = pr * 2 + jj
                                    for tt in range(2):
                                        tsl = slice(tt * 512, (tt + 1) * 512)
                                        p_g, p_u = pg[n % 2], pu[n % 2]
                                        s_ = sg[n % 2]
                                        n += 1
                                        for (pt, wt) in ((p_g, gt), (p_u, ut)):
                                            for kc in range(KC):
                                                c0 = kc * 256 + jj * 128
                                                self.mm(pt[:], wt[:, c0:c0 + 128], hTh[:, kc, tsl], kc == 0, kc == KC - 1,
                                                        reads=wt.b + [hTh.b[tt]], writes=pt.b)
                                        self.act(s_[:], p_g[:], AF.Silu, reads=p_g.b, writes=s_.b)
                                        if moe:
                                            self.tt(s_[:], s_[:], wB[:, e, tsl], ALU.mult, reads=s_.b + [wB.b[tt]], writes=s_.b)
                                        self.tt(hb[:, fl, tsl], p_u[:], s_[:], ALU.mult, reads=p_u.b + s_.b, writes=[hb.b[tt]])
                            for pr in range(FG // 2):
                                f0 = fg * FG + pr * 2
                                dt_ = wp[wi % 4]; wi += 1
                                src = Wd[f0 * 128:(f0 + 2) * 128, :].rearrange("(c p) n -> p c n", p=128)
                                S.dma("pool", dt_[:].rearrange("p (c n) -> p c n", c=2), src, writes=dt_.b)
                                dts.append(dt_)
                                if len(dts) == 2 or pr == FG // 2 - 1:
                                    pass
                            self._down_group(dts, hb, acc, pd, first, FG)
                            first = False
                    hsl = slice(half * TG, (half + 1) * TG)
                    for m in range(KC):
                        xr, xn = xres[m % 2], xnew[m % 2]
                        S.dma("sp", xr[:], self.R[R_in][:, m, hsl], reads=[self.Rb[R_in][m]], writes=xr.b)
                        self.stt(xn[:], acc[:, m, :], G[:, m:m + 1], xr[:], ALU.mult, ALU.add,
                                 reads=[acc.b[2 * m], acc.b[2 * m + 1]] + xr.b, writes=xn.b)
                        S.dma("sp", self.R[R_out][:, m, hsl], xn[:], reads=xn.b, writes=[self.Rb[R_out][m]])
            S.barrier()

    def _down_group(self, dts, hb, acc, pd, first, FG):
        if not hasattr(self, "_nd"):
            self._nd = 0
        for m in range(KC):
            for tt in range(2):
                tsl = slice(tt * 512, (tt + 1) * 512)
                p = pd[self._nd % 4]
                self._nd += 1
                for fl in range(FG):
                    d = dts[fl // 2]
                    c0 = (fl % 2) * D + m * 128
                    self.mm(p[:], d[:, c0:c0 + 128], hb[:, fl, tsl], fl == 0, fl == FG - 1, reads=d.b + [hb.b[tt]], writes=p.b)
                ab = [acc.b[2 * m + tt]]
                if first:
                    self.copy("dve", acc[:, m, tsl], p[:], reads=p.b, writes=ab)
                else:
                    self.tt(acc[:, m, tsl], p[:], acc[:, m, tsl], ALU.add, reads=p.b + ab, writes=ab)

    def moe_norm_route(self, hTh, wB, R_in, A, B, half):
        S, I = self.S, self.I
        with contextlib.ExitStack() as es:
            rw = self.sb(es, [128, KC, NE], F32)
            rb = self.sb(es, [128, NE], F32)
            h32 = [self.sb(es, [128, 512], F32) for _ in range(2)]
            lg = self.sb(es, [128, 4, NE], F32)
            mx8 = self.sb(es, [128, 8], F32)
            ntop = self.sb(es, [128, 1], F32)
            ex = self.sb(es, [128, NE], F32)
            msk = self.sb(es, [128, NE], F32)
            den = self.sb(es, [128, 1], F32)
            wts = self.sb(es, [128, 4, NE], F32)
            diag = [self.sb(es, [128, 128], F32) for _ in range(2)]
            ps_lg = [self.ps(es, [128, NE], F32) for _ in range(4)]
            ps_wb = [self.ps(es, [128, 512], F32) for _ in range(2)]
            S.dma("sp", rw[:], I["router_w"], writes=rw.b)
            S.dma("sp", rb[:], I["router_b"], writes=rb.b)
            state = {"n": 0}

            def h32_cb(kc, tmp, bias, out_ap, out_bufs):
                h = h32[state["n"] % 2]
                state["n"] += 1
                self.act(h[:], tmp[:], AF.Identity, reads=tmp.b, writes=h.b, bias=bias, scale=1.0)
                for s in range(4):
                    self.mm(ps_lg[s][:], h[:, s * 128:(s + 1) * 128], rw[:, kc, :], kc == 0, kc == KC - 1,
                            reads=h.b + rw.b, writes=ps_lg[s].b)
                self.copy("dve", out_ap, h[:], reads=h.b, writes=out_bufs)

            def tt_done(ti):
                for s in range(4):
                    self.tt(lg[:, s, :], ps_lg[s][:], rb[:], ALU.add, reads=ps_lg[s].b + rb.b, writes=lg.b)
                    S.add("dve", lambda e, s=s: e.max(mx8[:], lg[:, s, :]), reads=lg.b, writes=mx8.b)
                    self.ts(msk[:], lg[:, s, :], mx8[:, 1:2], None, ALU.is_ge, None, reads=lg.b + mx8.b, writes=msk.b)
                    self.ts(ntop[:], mx8[:, 0:1], -1.0, None, ALU.mult, None, reads=mx8.b, writes=ntop.b)
                    self.act(ex[:], lg[:, s, :], AF.Exp, reads=lg.b + ntop.b, writes=ex.b, bias=ntop[:], scale=1.0)
                    self.tt(ex[:], ex[:], msk[:], ALU.mult, reads=ex.b + msk.b, writes=ex.b)
                    S.add("dve", lambda e: e.reduce_sum(den[:], ex[:], AX.X), reads=ex.b, writes=den.b)
                    S.add("dve", lambda e: e.reciprocal(den[:], den[:]), reads=den.b, writes=den.b)
                    self.ts(wts[:, s, :], ex[:], den[:, 0:1], None, ALU.mult, None, reads=ex.b + den.b, writes=wts.b)
                k = 0
                for e_ in range(NE):
                    p = ps_wb[e_ % 2]
                    for s in range(4):
                        dg = diag[k % 2]
                        k += 1
                        self.ts(dg[:], self.ident[:], wts[:, s, e_:e_ + 1], None, ALU.mult, None, reads=wts.b, writes=dg.b)
                        self.mm(p[:, s * 128:(s + 1) * 128], self.ones32[:], dg[:], True, True, reads=dg.b, writes=p.b)
                    self.copy("act", wB[:, e_, ti * 512:(ti + 1) * 512], p[:], reads=p.b, writes=[wB.b[ti]])

            self.phase_prenorm(hTh, False, R_in, None, A, B, t_lo=half * 2, n_tt=2, h32_cb=h32_cb, tt_done_cb=tt_done)


    def phase_moe_sparse(self, R_in, A, B, G, experts):
        S, I = self.S, self.I
        CAP = 896
        BIG = 4096.0
        XG = [self.dscr(f"XG{e}", [CAP, D], BF16) for e in range(NE)]
        YG = [self.dscr(f"YG{e}", [CAP, D], F32) for e in range(NE)]
        XGz = [Buf() for _ in range(NE)]
        YGb = [Buf() for _ in range(NE)]
        I32 = mybir.dt.int32
        with contextlib.ExitStack() as es0:
            wts_all = self.sb(es0, [128, 16, NE], F32)
            idx_all = self.sb(es0, [128, 16, NE], I32)
            with contextlib.ExitStack() as es:
                rw = self.sb(es, [128, KC, NE], F32)
                rb = self.sb(es, [128, NE], F32)
                us = self.sb(es, [128, 128], F32)
                zt = self.sb(es, [128, D], BF16)
                run = self.sb(es, [128, NE], F32)
                h32 = [self.sb(es, [128, 512], F32) for _ in range(2)]
                hTt = self.sb(es, [128, KC, 512], BF16, nb=1)
                htok = [self.sb(es, [128, D], BF16) for _ in range(4)]
                lg = self.sb(es, [128, NE], F32)
                mx8 = self.sb(es, [128, 8], F32)
                ntop = self.sb(es, [128, 1], F32)
                ex = self.sb(es, [128, NE], F32)
                msk = self.sb(es, [128, NE], F32)
                den = self.sb(es, [128, 1], F32)
                posf = self.sb(es, [128, NE], F32)
                ps_lg = [self.ps(es, [128, NE], F32) for _ in range(4)]
                ps_pos = self.ps(es, [128, 2 * NE], F32)
                ps_th = [self.ps(es, [128, 1024], BF16) for _ in range(2)]
                S.dma("sp", rw[:], I["router_w"], writes=rw.b)
                S.dma("sp", rb[:], I["router_b"], writes=rb.b)
                S.dma("sp", us[:], I["ustrict"], writes=us.b)
                S.add("dve", lambda e: e.memset(zt[:], 0.0), writes=zt.b)
                S.add("dve", lambda e: e.memset(run[:], 0.0), writes=run.b)
                for e_ in range(NE):
                    for st in range(CAP // 128):
                        S.dma("sp", XG[e_][st * 128:(st + 1) * 128, :], zt[:], reads=zt.b, writes=[XGz[e_]])
                state = {"n": 0, "nth": 0}

                def h32_cb(kc, tmp, bias, out_ap, out_bufs):
                    h = h32[state["n"] % 2]
                    state["n"] += 1
                    self.act(h[:], tmp[:], AF.Identity, reads=tmp.b, writes=h.b, bias=bias, scale=1.0)
                    for s_ in range(4):
                        self.mm(ps_lg[s_][:], h[:, s_ * 128:(s_ + 1) * 128], rw[:, kc, :], kc == 0, kc == KC - 1,
                                reads=h.b + rw.b, writes=ps_lg[s_].b)
                    self.copy("dve", out_ap, h[:], reads=h.b, writes=out_bufs)

                def tt_done(ti):
                    for s_ in range(4):
                        g = ti * 4 + s_
                        self.tt(lg[:], ps_lg[s_][:], rb[:], ALU.add, reads=ps_lg[s_].b + rb.b, writes=lg.b)
                        S.add("dve", lambda e: e.max(mx8[:], lg[:]), reads=lg.b, writes=mx8.b)
                        self.ts(msk[:], lg[:], mx8[:, 1:2], None, ALU.is_ge, None, reads=lg.b + mx8.b, writes=msk.b)
                        self.ts(ntop[:], mx8[:, 0:1], -1.0, None, ALU.mult, None, reads=mx8.b, writes=ntop.b)
                        self.act(ex[:], lg[:], AF.Exp, reads=lg.b + ntop.b, writes=ex.b, bias=ntop[:], scale=1.0)
                        self.tt(ex[:], ex[:], msk[:], ALU.mult, reads=ex.b + msk.b, writes=ex.b)
                        S.add("dve", lambda e: e.reduce_sum(den[:], ex[:], AX.X), reads=ex.b, writes=den.b)
                        S.add("dve", lambda e: e.reciprocal(den[:], den[:]), reads=den.b, writes=den.b)
                        self.ts(wts_all[:, g, :], ex[:], den[:, 0:1], None, ALU.mult, None, reads=ex.b + den.b, writes=wts_all.b)
                        self.mm(ps_pos[:, 0:NE], us[:], msk[:], True, True, reads=us.b + msk.b, writes=ps_pos.b)
                        self.mm(ps_pos[:, NE:2 * NE], self.ones32[:], msk[:], True, True, reads=msk.b, writes=ps_pos.b)
                        self.tt(posf[:], ps_pos[:, 0:NE], run[:], ALU.add, reads=ps_pos.b + run.b, writes=posf.b)
                        self.stt(posf[:], posf[:], -BIG, msk[:], ALU.add, ALU.mult, reads=posf.b + msk.b, writes=posf.b)
                        self.ts(posf[:], posf[:], BIG, None, ALU.add, None, reads=posf.b, writes=posf.b)
                        self.copy("dve", idx_all[:, g, :], posf[:], reads=posf.b, writes=idx_all.b)
                        self.tt(run[:], run[:], ps_pos[:, NE:2 * NE], ALU.add, reads=run.b + ps_pos.b, writes=run.b)
                        ht = htok[s_]
                        for hf in range(2):
                            p = ps_th[state["nth"] % 2]
                            state["nth"] += 1
                            for i in range(8):
                                kc = hf * 8 + i
                                self.tr(p[:, i * 128:(i + 1) * 128], hTt[:, kc, s_ * 128:(s_ + 1) * 128], self.identb[:],
                                        reads=hTt.b, writes=p.b)
                            self.copy("act", ht[:, hf * 1024:(hf + 1) * 1024], p[:], reads=p.b, writes=ht.b)
                        for e_ in range(NE):
                            off = bass.IndirectOffsetOnAxis(ap=idx_all[:, g, e_:e_ + 1], axis=0)
                            S.add("pool", lambda eng, e_=e_, off=off, ht=ht: eng.indirect_dma_start(
                                out=XG[e_][:, :], out_offset=off, in_=ht[:, :], in_offset=None,
                                bounds_check=self._bc_reg(eng, CAP - 1), oob_is_err=False),
                                reads=ht.b + idx_all.b + [XGz[e_]], writes=(), dma=True)

                self.phase_prenorm(_HTView(hTt), False, R_in, None, A, B, t_lo=0, n_tt=4, h32_cb=h32_cb, tt_done_cb=tt_done)
            S.barrier()
            FG = 8
            with contextlib.ExitStack() as es2:
                xgT = self.sb(es2, [128, KC, CAP], BF16, nb=2)
                acc = self.sb(es2, [128, CAP // 128, D], F32, nb=32)
                hid = [self.sb(es2, [128, FG, CAP], BF16, nb=2) for _ in range(2)]
                NW = 4
                wp = [self.sb(es2, [128, KC * 512], BF16) for _ in range(NW)]
                sg = [self.sb(es2, [128, 512], F32) for _ in range(2)]
                xs = [self.sb(es2, [128, D], BF16) for _ in range(2)]
                pg = [self.ps(es2, [128, 512], F32) for _ in range(2)]
                pu = [self.ps(es2, [128, 512], F32) for _ in range(2)]
                pd = [self.ps(es2, [128, 512], F32) for _ in range(3)]
                pt = self.ps(es2, [128, 1024], BF16)
                wi = 0
                n = 0
                nd = 0
                for e_, (Wg, Wu, Wd, F) in enumerate(experts):
                    for st in range(CAP // 128):
                        x_ = xs[st % 2]
                        S.dma("sp", x_[:], XG[e_][st * 128:(st + 1) * 128, :], writes=x_.b)
                        for hf in range(2):
                            for i in range(8):
                                kc = hf * 8 + i
                                self.tr(pt[:, i * 128:(i + 1) * 128], x_[:, kc * 128:(kc + 1) * 128], self.identb[:], reads=x_.b, writes=pt.b)
                            self.copy("act" if hf else "dve", xgT[:, hf * 8:(hf + 1) * 8, st * 128:(st + 1) * 128],
                                      pt[:].rearrange("p (a b) -> p a b", a=8), reads=pt.b, writes=[xgT.b[st // 4]])
                    for fg in range(F // FG):
                        hb = hid[fg % 2]
                        dts = []
                        for pr in range(FG // 4):
                            f0 = fg * FG + pr * 4
                            gt = wp[wi % NW]; wi += 1
                            ut = wp[wi % NW]; wi += 1
                            for (wt, W) in ((gt, Wg), (ut, Wu)):
                                src = W[:, f0 * 128:(f0 + 4) * 128].rearrange("(kc p) n -> p kc n", p=128)
                                S.dma("pool", wt[:].rearrange("p (kc n) -> p kc n", kc=KC), src, writes=wt.b)
                            for jj in range(4):
                                fl = pr * 4 + jj
                                for tt in range(2):
                                    tsl = slice(tt * 512, min((tt + 1) * 512, CAP))
                                    tw = tsl.stop - tsl.start
                                    p_g, p_u = pg[n % 2], pu[n % 2]
                                    s_ = sg[n % 2]
                                    n += 1
                                    for (pt_, wt) in ((p_g, gt), (p_u, ut)):
                                        for kc in range(KC):
                                            c0 = kc * 512 + jj * 128
                                            self.mm(pt_[:, 0:tw], wt[:, c0:c0 + 128], xgT[:, kc, tsl], kc == 0, kc == KC - 1,
                                                    reads=wt.b + [xgT.b[tt]], writes=pt_.b)
                                    self.act(s_[:, 0:tw], p_g[:, 0:tw], AF.Silu, reads=p_g.b, writes=s_.b)
                                    self.tt(hb[:, fl, tsl], p_u[:, 0:tw], s_[:, 0:tw], ALU.mult, reads=p_u.b + s_.b, writes=[hb.b[tt]])
                        for pr in range(FG // 4):
                            f0 = fg * FG + pr * 4
                            dt_ = wp[wi % NW]; wi += 1
                            src = Wd[f0 * 128:(f0 + 4) * 128, :].rearrange("(c p) n -> p c n", p=128)
                            S.dma("pool", dt_[:].rearrange("p (c n) -> p c n", c=4), src, writes=dt_.b)
                            dts.append(dt_)
                        for st in range(CAP // 128):
                            for q in range(4):
                                p = pd[nd % 3]
                                nd += 1
                                for fl in range(FG):
                                    d = dts[fl // 4]
                                    c0 = (fl % 4) * D + q * 512
                                    self.mm(p[:], hb[:, fl, st * 128:(st + 1) * 128], d[:, c0:c0 + 512], fl == 0, fl == FG - 1,
                                            reads=d.b + [hb.b[st // 4]], writes=p.b)
                                ab = [acc.b[st * 4 + q]]
                                dst = acc[:, st, q * 512:(q + 1) * 512]
                                if fg == 0:
                                    self.copy("dve", dst, p[:], reads=p.b, writes=ab)
                                else:
                                    self.tt(dst, p[:], dst, ALU.add, reads=p.b + ab, writes=ab)
                    S.dma("sp", YG[e_].rearrange("(st p) f -> p st f", p=128), acc[:], reads=acc.b, writes=[YGb[e_]])
            S.barrier()
            with contextlib.ExitStack() as es3:
                GfB = self.sb(es3, [128, D], F32)
                diag = [self.sb(es3, [128, 128], F32) for _ in range(2)]
                Gt = [self.sb(es3, [128, D], F32) for _ in range(3)]
                at = [self.sb(es3, [128, D], F32) for _ in range(2)]
                xTt = [self.sb(es3, [128, KC, 128], F32) for _ in range(2)]
                px = [self.ps(es3, [128, 512], F32) for _ in range(8)]
                for kc in range(KC):
                    dg = diag[kc % 2]
                    p = px[kc % 8]
                    self.ts(dg[:], self.ident[:], G[:, kc:kc + 1], None, ALU.mult, None, reads=(), writes=dg.b)
                    self.mm(p[:, 0:128], self.ones32[:], dg[:], True, True, reads=dg.b, writes=p.b)
                    self.copy("act", GfB[:, kc * 128:(kc + 1) * 128], p[:, 0:128], reads=p.b, writes=GfB.b)
                for g_ in Gt:
                    S.add("dve", lambda e, g_=g_: e.memset(g_[:], 0.0), writes=g_.b)
                ob = Buf()
                ng = 0
                for g in range(16):
                    x_ = xTt[g % 2]
                    a_ = at[g % 2]
                    S.dma("sp", x_[:], self.R[R_in][:, :, g * 128:(g + 1) * 128], reads=self.Rb[R_in], writes=x_.b)
                    pq = px[(g % 2) * 4:(g % 2) * 4 + 4]
                    for q in range(4):
                        for i in range(4):
                            kc = q * 4 + i
                            self.tr(pq[q][:, i * 128:(i + 1) * 128], x_[:, kc, :], self.ident[:], reads=x_.b, writes=pq[q].b)
                    for e_ in range(NE):
                        gt_ = Gt[ng % 3]
                        ng += 1
                        off = bass.IndirectOffsetOnAxis(ap=idx_all[:, g, e_:e_ + 1], axis=0)
                        S.add("pool", lambda eng, e_=e_, off=off, gt_=gt_: eng.indirect_dma_start(
                            out=gt_[:, :], out_offset=None, in_=YG[e_][:, :], in_offset=off,
                            bounds_check=self._bc_reg(eng, CAP - 1), oob_is_err=False),
                            reads=[YGb[e_]], writes=gt_.b, dma=True)
                        if e_ == 0:
                            self.ts(a_[:], gt_[:], wts_all[:, g, 0:1], None, ALU.mult, None, reads=gt_.b, writes=a_.b)
                        else:
                            self.stt(a_[:], gt_[:], wts_all[:, g, e_:e_ + 1], a_[:], ALU.mult, ALU.add, reads=gt_.b + a_.b, writes=a_.b)
                    self.tt(a_[:], a_[:], GfB[:], ALU.mult, reads=a_.b + GfB.b, writes=a_.b)
                    for q in range(4):
                        qs_ = slice(q * 512, (q + 1) * 512)
                        self.tt(a_[:, qs_], pq[q][:], a_[:, qs_], ALU.add, reads=pq[q].b + a_.b, writes=a_.b)
                    S.dma("sp", self.out[g * 128:(g + 1) * 128, :], a_[:], reads=a_.b, writes=[ob])

    def phase_attn(self, R_in, R_out):
        S, I = self.S, self.I
        qs = self.dscr("qs", [NH, 128, T], BF16)
        ks = self.dscr("ks", [NH, 128, T], BF16)
        qsb = [Buf() for _ in range(NH)]
        ksb = [Buf() for _ in range(NH)]
        with contextlib.ExitStack() as es:
            vtok = self.sb(es, [128, 16, D], BF16)
            selb = self.sb(es, [128, NH, 8, 8], F32)
            with contextlib.ExitStack() as es1:
                hT = self.sb(es1, [128, KC, T], BF16, nb=4)
                self.phase_prenorm(hT, False, R_in, None, self.Aprm[:, 1, 0, :], self.modT[:, 1, 0:16], nbuf=1)
                S.barrier()
                self.attn_qkv(hT, vtok, selb, qs, ks, qsb, ksb)
                S.barrier()
            with contextlib.ExitStack() as es2:
                oT = self.sb(es2, [128, KC, T], BF16)
                self.attn_core(vtok, selb, qs, ks, qsb, ksb, oT)
                S.barrier()
                self.phase_outproj(oT, I["attn_out"], self.modT[:, 1, 32:48], R_in, R_out)

    def attn_qkv(self, hT, vtok, selb, qs, ks, qsb, ksb):
        S, I = self.S, self.I
        W = I["qkv_w"]
        with contextlib.ExitStack() as es:
            wp = [self.sb(es, [128, KC, 128], BF16) for _ in range(6)]
            rope = self.sb(es, [128, 2, T], F32)
            qkg = self.sb(es, [128, 4], F32)
            pmask = self.sb(es, [128, 8, 8], F32)
            NB = 2
            x32 = [self.sb(es, [128, 512], F32) for _ in range(NB)]
            rs = [self.sb(es, [128, 512], F32) for _ in range(NB)]
            xn = [self.sb(es, [128, 512], F32) for _ in range(NB)]
            xsw = [self.sb(es, [128, 512], F32) for _ in range(NB)]
            t2 = [self.sb(es, [128, 512], F32) for _ in range(NB)]
            xb = [self.sb(es, [128, 512], BF16) for _ in range(NB)]
            sqh = [self.sb(es, [128, 512], BF16) for _ in range(NB)]
            kmean = [self.sb(es, [128, NBLK], F32) for _ in range(2)]
            gm = self.sb(es, [128, 8, 8], F32)
            mx8 = self.sb(es, [128, 8], F32)
            sel = self.sb(es, [128, 8], F32)
            pp = [self.ps(es, [128, 512], F32) for _ in range(3)]
            ps_s = [self.ps(es, [128, 512], F32) for _ in range(2)]
            ps_v = [self.ps(es, [128, 128], F32) for _ in range(2)]
            ps_gate = self.ps(es, [128, 8, 8], F32)
            S.dma("sp", rope[:], I["ropeT"], writes=rope.b)
            S.dma("sp", qkg[:], I["qk_gain"], writes=qkg.b)
            S.dma("sp", pmask[:], I["pastmask"], writes=pmask.b)
            units = [(h, which, tt) for h in range(NH) for which in ("k", "q") for tt in range(4)]
            NU = len(units)
            st = {"nv": 0}

            def wts(h):
                base = (h % 2) * 3
                return wp[base], wp[base + 1], wp[base + 2]

            def P1(u):
                h, which, tt = units[u]
                wq, wk, wv = wts(h)
                if which == "k" and tt == 0:
                    for k_, w_ in enumerate((wq, wk, wv)):
                        self.wload(w_, W, k_ * D + h * 128, 128)
                w = wk if which == "k" else wq
                tsl = slice(tt * 512, (tt + 1) * 512)
                p = pp[u % 3]
                for kc in range(KC):
                    self.mm(p[:], w[:, kc, :], hT[:, kc, tsl], kc == 0, kc == KC - 1, reads=w.b + [hT.b[tt]], writes=p.b)
                vi = (0 if which == "k" else 4) + tt
                for tk in (2 * vi, 2 * vi + 1):
                    pv = ps_v[st["nv"] % 2]
                    st["nv"] += 1
                    for kc in range(KC):
                        self.mm(pv[:], hT[:, kc, tk * 128:(tk + 1) * 128], wv[:, kc, :], kc == 0, kc == KC - 1,
                                reads=[hT.b[tk // 4]] + wv.b, writes=pv.b)
                    self.copy("act" if tk % 2 else "dve", vtok[:, tk, h * 128:(h + 1) * 128], pv[:], reads=pv.b, writes=vtok.b)
                x_ = x32[u % NB]
                self.copy("dve", x_[:], p[:], reads=p.b, writes=x_.b)
                self.act(sqh[u % NB][:], x_[:], AF.Square, reads=x_.b, writes=sqh[u % NB].b)

            def P2(u):
                h, which, tt = units[u]
                gi = 1 if which == "k" else 0
                p2 = ps_s[u % 2]
                x_, r_, n_, w_ = x32[u % NB], rs[u % NB], xn[u % NB], xsw[u % NB]
                self.mm(p2[:], self.onesH[:], sqh[u % NB][:], True, True, reads=sqh[u % NB].b, writes=p2.b)
                self.act(r_[:], p2[:], AF.Ln, reads=p2.b, writes=r_.b, bias=self.epsT[:], scale=1.0)
                self.act(r_[:], r_[:], AF.Exp, reads=r_.b, writes=r_.b, scale=-0.5)
                self.stt(n_[:], x_[:], qkg[:, gi:gi + 1], r_[:], ALU.mult, ALU.mult, reads=x_.b + r_.b + qkg.b, writes=n_.b)
                S.dma("sp", w_[0:64, :], n_[64:128, :], reads=n_.b, writes=w_.b)
                S.dma("sp", w_[64:128, :], n_[0:64, :], reads=n_.b, writes=w_.b)

            def P3(u):
                h, which, tt = units[u]
                tsl = slice(tt * 512, (tt + 1) * 512)
                r_, n_, w_, t_, b_ = rs[u % NB], xn[u % NB], xsw[u % NB], t2[u % NB], xb[u % NB]
                kr = r_
                self.tt(t_[:], w_[:], rope[:, 1, tsl], ALU.mult, reads=w_.b + rope.b, writes=t_.b)
                self.tt(n_[:], n_[:], rope[:, 0, tsl], ALU.mult, reads=n_.b + rope.b, writes=n_.b)
                self.tt(kr[:], n_[:], t_[:], ALU.add, reads=n_.b + t_.b, writes=kr.b)
                self.copy("act", b_[:], kr[:], reads=kr.b, writes=b_.b)
                km = kmean[h % 2]
                if which == "k":
                    S.dma("sp", ks[h][:, tsl], b_[:], reads=b_.b, writes=[ksb[h]])
                    S.add("dve", lambda e: e.reduce_sum(km[:, 2 * tt:2 * tt + 2], kr[:].rearrange("p (n k) -> p n k", n=2), AX.X),
                          reads=kr.b, writes=km.b)
                    if tt == 3:
                        self.ts(km[:], km[:], 1.0 / BLK, None, ALU.mult, None, reads=km.b, writes=km.b)
                else:
                    S.dma("sp", qs[h][:, tsl], b_[:], reads=b_.b, writes=[qsb[h]])
                    if tt >= 2:
                        for i in range(4):
                            qi = (tt - 2) * 4 + i
                            self.mm(ps_gate[:, qi, :], kr[:, i * 128:(i + 1) * 128], km[:], True, True,
                                    reads=kr.b + km.b, writes=ps_gate.b)
                    if tt == 3:
                        self.tt(gm[:], ps_gate[:], pmask[:], ALU.add, reads=ps_gate.b + pmask.b, writes=gm.b)
                        for qi in range(8):
                            S.add("dve", lambda e, qi=qi: e.max(mx8[:], gm[:, qi, :]), reads=gm.b, writes=mx8.b)
                            self.ts(sel[:], gm[:, qi, :], mx8[:, 2:3], None, ALU.is_ge, None, reads=gm.b + mx8.b, writes=sel.b)
                            self.ts(selb[:, h, qi, :], sel[:], -1.0, -NEG, ALU.add, ALU.mult, reads=sel.b, writes=selb.b)

            for i in range(NU + 2):
                if i < NU:
                    P1(i)
                if 0 <= i - 1 < NU:
                    P2(i - 1)
                if 0 <= i - 2 < NU:
                    P3(i - 2)

    def attn_core(self, vtok, selb, qs, ks, qsb, ksb, oT):
        S, I = self.S, self.I
        with contextlib.ExitStack() as es:
            tri = self.sb(es, [128, 256], F32)
            qT = [self.sb(es, [128, T], BF16) for _ in range(2)]
            kT = [self.sb(es, [128, T], BF16) for _ in range(2)]
            P = [self.sb(es, [128, T], BF16) for _ in range(3)]
            PT = [self.sb(es, [128, T], BF16) for _ in range(2)]
            rsum = [self.sb(es, [128, 16], F32) for _ in range(3)]
            rtot = [self.sb(es, [128, 1], F32) for _ in range(2)]
            stri = [self.sb(es, [128, 256], F32) for _ in range(2)]
            otok = [self.sb(es, [128, 128], BF16) for _ in range(2)]
            ps_S = [self.ps(es, [128, 256], F32) for _ in range(2)]
            ps_PT = [self.ps(es, [128, 512], BF16) for _ in range(2)]
            ps_O = [self.ps(es, [128, 128], F32) for _ in range(2)]
            ps_OT = [self.ps(es, [128, 128], BF16) for _ in range(2)]
            S.dma("sp", tri[:], I["tri"], writes=tri.b)
            iters = [(h, qt) for h in range(NH) for qt in range(16)]
            N = len(iters)
            st = {"nS": 0, "nPT": 0}
            nkts = {}

            def stageA(it):
                h, qt = iters[it]
                q_, k_ = qT[h % 2], kT[h % 2]
                if qt == 0:
                    S.dma("sp", q_[:], qs[h], reads=[qsb[h]], writes=q_.b)
                    S.dma("sp", k_[:], ks[h], reads=[ksb[h]], writes=k_.b)
                own, half = qt // 2, qt % 2
                P_, rs_ = P[it % 3], rsum[it % 3]
                qsl = slice(qt * 128, (qt + 1) * 128)
                S.add("dve", lambda e: e.memset(rs_[:], 0.0), writes=rs_.b)
                col = 0
                nop = 0
                for nb in range(own):
                    p = ps_S[st["nS"] % 2]
                    st["nS"] += 1
                    self.mm(p[:], q_[:, qsl], k_[:, nb * 256:(nb + 1) * 256], True, True, reads=q_.b + k_.b, writes=p.b)
                    bias = selb[:, h, qt - 8, nb:nb + 1] if own >= 4 else None
                    self.act(P_[:, col:col + 256], p[:], AF.Exp, reads=p.b + selb.b, writes=P_.b + rs_.b, bias=bias, scale=ATT_SCALE,
                             accum_out=rs_[:, nop:nop + 1])
                    col += 256
                    nop += 1
                nk = 128 if half == 0 else 256
                p = ps_S[st["nS"] % 2]
                st_ = stri[st["nS"] % 2]
                st["nS"] += 1
                self.mm(p[:, 0:nk], q_[:, qsl], k_[:, own * 256:own * 256 + nk], True, True, reads=q_.b + k_.b, writes=p.b)
                msk = tri[:, 128:256] if half == 0 else tri[:, 0:256]
                self.tt(st_[:, 0:nk], p[:, 0:nk], msk, ALU.add, reads=p.b + tri.b, writes=st_.b)
                self.act(P_[:, col:col + nk], st_[:, 0:nk], AF.Exp, reads=st_.b, writes=P_.b + rs_.b, scale=ATT_SCALE,
                         accum_out=rs_[:, nop:nop + 1])
                col += nk
                nkts[it] = col // 128

            def stageB(it):
                P_, PT_ = P[it % 3], PT[it % 2]
                nkt = nkts[it]
                for g0 in range(0, nkt, 4):
                    pp_ = ps_PT[st["nPT"] % 2]
                    st["nPT"] += 1
                    gn = min(4, nkt - g0)
                    for i in range(gn):
                        kt = g0 + i
                        self.tr(pp_[:, i * 128:(i + 1) * 128], P_[:, kt * 128:(kt + 1) * 128], self.identb[:], reads=P_.b, writes=pp_.b)
                    self.copy("dve", PT_[:, g0 * 128:(g0 + gn) * 128], pp_[:, 0:gn * 128], reads=pp_.b, writes=PT_.b)

            def stageC(it):
                h, qt = iters[it]
                PT_, rs_, rt_ = PT[it % 2], rsum[it % 3], rtot[it % 2]
                ot_, pO, pOT = otok[it % 2], ps_O[it % 2], ps_OT[it % 2]
                nkt = nkts[it]
                qsl = slice(qt * 128, (qt + 1) * 128)
                for kt in range(nkt):
                    self.mm(pO[:], PT_[:, kt * 128:(kt + 1) * 128], vtok[:, kt, h * 128:(h + 1) * 128], kt == 0, kt == nkt - 1,
                            reads=PT_.b + vtok.b, writes=pO.b)
                S.add("dve", lambda e: e.reduce_sum(rt_[:], rs_[:], AX.X), reads=rs_.b, writes=rt_.b)
                S.add("dve", lambda e: e.reciprocal(rt_[:], rt_[:]), reads=rt_.b, writes=rt_.b)
                self.act(ot_[:], pO[:], AF.Identity, reads=pO.b + rt_.b, writes=ot_.b, scale=rt_[:, 0:1])
                self.tr(pOT[:], ot_[:], self.identb[:], reads=ot_.b, writes=pOT.b)
                self.copy("dve", oT[:, h, qsl], pOT[:], reads=pOT.b, writes=oT.b)

            for i in range(N + 2):
                if i < N:
                    stageA(i)
                if 0 <= i - 1 < N:
                    stageB(i - 1)
                if 0 <= i - 2 < N:
                    stageC(i - 2)

    def phase_final(self, cur):
        S = self.S
        with contextlib.ExitStack() as es:
            xT = [self.sb(es, [128, KC, 512], F32) for _ in range(2)]
            xo = [self.sb(es, [128, D], F32) for _ in range(2)]
            ps_tr = [self.ps(es, [128, 512], F32) for _ in range(4)]
            n = 0
            no = 0
            ob = Buf()
            for tt in range(4):
                x_ = xT[tt % 2]
                S.dma("sp", x_[:], self.R[cur][:, :, tt * 512:(tt + 1) * 512], reads=self.Rb[cur], writes=x_.b)
                for s in range(4):
                    o_ = xo[no % 2]
                    no += 1
                    for c4 in range(4):
                        p = ps_tr[n % 4]
                        n += 1
                        for i in range(4):
                            kc = c4 * 4 + i
                            self.tr(p[:, i * 128:(i + 1) * 128], x_[:, kc, s * 128:(s + 1) * 128], self.ident[:], reads=x_.b, writes=p.b)
                        self.copy("act" if c4 % 2 else "dve", o_[:, c4 * 512:(c4 + 1) * 512], p[:], reads=p.b, writes=o_.b)
                    r0 = tt * 512 + s * 128
                    S.dma("sp", self.out[r0:r0 + 128, :], o_[:], reads=o_.b, writes=[ob])


def _pk(v):
    v = np.asarray(v, dtype=np.float32)
    return np.ascontiguousarray(v.reshape(-1, 128).T)


def _consts():
    half = HD // 2
    inv_freq = np.exp(-math.log(10000.0) * np.arange(half, dtype=np.float32) / half).astype(np.float32)
    ang = np.arange(T, dtype=np.float32)[:, None] * inv_freq[None, :]
    cos = np.cos(ang).astype(np.float32).T
    sin = np.sin(ang).astype(np.float32).T
    ropeT = np.zeros((128, 2, T), np.float32)
    ropeT[:half, 0] = cos
    ropeT[half:, 0] = cos
    ropeT[:half, 1] = -sin
    ropeT[half:, 1] = sin
    tri = np.zeros((128, 256), np.float32)
    qi = np.arange(128)[:, None]
    ki = np.arange(128)[None, :]
    tri[:, 128:] = np.where(ki <= qi, 0.0, -1e9).astype(np.float32)
    pm = np.zeros((128, 8, 8), np.float32)
    for i in range(8):
        own = (8 + i) // 2
        pm[:, i, own:] = -1e30
    return ropeT, tri, pm


def make_in_map(inputs, b, stop=None):
    ropeT, tri, pm = _consts()
    m = {}
    m["x"] = np.ascontiguousarray(inputs["x"][b])
    m["cT"] = _pk(inputs["c"][b])
    m["mod_w"] = np.asarray(inputs["mod_w"])
    mb = np.asarray(inputs["mod_b"], np.float32)
    m["mod_bT"] = np.ascontiguousarray(np.stack([_pk(mb[0]), _pk(mb[1])], axis=1))
    g = [inputs["norm_mix"][0], inputs["norm_ffn"][0], inputs["norm_mix"][1], inputs["norm_ffn"][1]]
    m["gains"] = np.ascontiguousarray(np.stack([_pk(v) for v in g], axis=1))
    m["ident"] = np.eye(128, dtype=np.float32)
    m["conv_in"] = np.asarray(inputs["conv_in"][0])
    cw = np.asarray(inputs["conv_w"][0], np.float32)
    m["conv_wT"] = np.ascontiguousarray(np.stack([_pk(cw[k]) for k in range(3)], axis=1))
    m["conv_out"] = np.asarray(inputs["conv_out"][0])
    m["ffn_gate"] = np.asarray(inputs["ffn_gate"][0])
    m["ffn_up"] = np.asarray(inputs["ffn_up"][0])
    m["ffn_down"] = np.asarray(inputs["ffn_down"][0])
    if stop is None or stop >= 3:
        m["qkv_w"] = np.asarray(inputs["qkv_w"][0])
        gq = np.asarray(inputs["q_norm"][0], np.float32)
        gk = np.asarray(inputs["k_norm"][0], np.float32)
        m["qk_gain"] = np.ascontiguousarray(np.stack([gq, gk, np.roll(gq, 64), np.roll(gk, 64)], axis=1))
        m["ropeT"] = ropeT
        m["tri"] = tri
        m["pastmask"] = pm
        m["attn_out"] = np.asarray(inputs["attn_out"][0])
    if stop is None or stop >= 4:
        rw = np.asarray(inputs["router_w"][0], np.float32)
        m["router_w"] = np.ascontiguousarray(rw.reshape(KC, 128, NE).transpose(1, 0, 2))
        m["router_b"] = np.ascontiguousarray(np.broadcast_to(np.asarray(inputs["router_b"][0], np.float32)[None, :], (128, NE)))
        m["ustrict"] = np.ascontiguousarray(np.triu(np.ones((128, 128), np.float32), 1))
        m["exp_gate"] = np.asarray(inputs["exp_gate"][0])
        m["exp_up"] = np.asarray(inputs["exp_up"][0])
        m["exp_down"] = np.asarray(inputs["exp_down"][0])
    return m


_CACHE = {}


def kernel(**inputs):
    if "nc" not in _CACHE:
        _CACHE["nc"] = Prog().build()
    nc = _CACHE["nc"]
    n = 8
    in_maps = [make_in_map(inputs, b) for b in range(n)]
    res = run_bass_kernel_spmd(nc, in_maps, core_ids=list(range(n)))
    return np.stack([np.asarray(r["out"], dtype=np.float32) for r in res.results], axis=0)
```
